# Optimizing a Trainium2 kernel written in Bass

```python
import math
import jax, jax.numpy as jnp
from jax import lax
import numpy as np

D_MODEL = 1024
BATCH = 8
SEQ = 2048
DEPTH = 1

GRID_W = 64
CTX_LEN = 256

D_MIX = D_MODEL
D_RNN = D_MIX // 2
D_HYENA = D_MIX - D_RNN
RNN_HEADS = 8
RNN_HEAD_DIM = D_RNN // RNN_HEADS
RNN_CONV_W = 4
RNN_CONV_LEFT = 2
LRU_C = 8.0
HYENA_ORDER = 2
HYENA_SHORT_W = 3
FILTER_EMB = 33
FILTER_HIDDEN = 64
DECAY_FAST_PCT = 0.3
DECAY_SLOW_PCT = 1.5
DECAY_TARGET = 1e-2
N_GROUPS = 4
EXPERTS_PER_GROUP = 8
N_EXPERTS = N_GROUPS * EXPERTS_PER_GROUP
TOP_K = 2
D_EXPERT = 512
N_MOD = 6
EPS = 1e-6
D_IN = 2 * D_RNN + (HYENA_ORDER + 1) * D_HYENA

kernel_name = "hybrid_rglru_hyena_hmoe_dit_block"

F32 = jnp.float32


def _rmsnorm(u, g):
    u32 = u.astype(F32)
    y = u32 * lax.rsqrt(jnp.mean(u32 * u32, axis=-1, keepdims=True) + EPS) * g.astype(F32)
    return y.astype(u.dtype)


def _modulate(h, shift, scale):
    return h * (1 + scale) + shift


def _sincos_2d(rows, cols, dim):
    quarter = dim // 4
    omega = 1.0 / (10000.0 ** (jnp.arange(quarter, dtype=F32) / quarter))
    ang_r = jnp.arange(rows, dtype=F32)[:, None] * omega
    ang_c = jnp.arange(cols, dtype=F32)[:, None] * omega
    emb_r = jnp.concatenate([jnp.sin(ang_r), jnp.cos(ang_r)], axis=-1)
    emb_c = jnp.concatenate([jnp.sin(ang_c), jnp.cos(ang_c)], axis=-1)
    pe = jnp.concatenate([jnp.broadcast_to(emb_r[:, None, :], (rows, cols, 2 * quarter)),
                          jnp.broadcast_to(emb_c[None, :, :], (rows, cols, 2 * quarter))], axis=-1)
    return pe.reshape(rows * cols, 4 * quarter)


def _dwconv(u, w, pad_left):
    k_w = w.shape[0]
    n = u.shape[1]
    up = jnp.pad(u, ((0, 0), (pad_left, k_w - 1 - pad_left), (0, 0)))
    y = up[:, 0:n] * w[0]
    for k in range(1, k_w):
        y = y + up[:, k:k + n] * w[k]
    return y


def _block_diag(u, w, b):
    bn, n, _ = u.shape
    uh = u.reshape(bn, n, RNN_HEADS, RNN_HEAD_DIM)
    return jnp.einsum("blhi,hij->blhj", uh, w).reshape(bn, n, D_RNN) + b


def _linear_scan(a, b, h0, reverse):
    if reverse:
        b = b.at[:, -1].add(a[:, -1] * h0)
    else:
        b = b.at[:, 0].add(a[:, 0] * h0)

    def combine(left, right):
        a_l, b_l = left
        a_r, b_r = right
        return a_l * a_r, a_r * b_l + b_r

    _, h = lax.associative_scan(combine, (a, b), axis=1, reverse=reverse)
    return h


def _rglru_scan(u, h0, wa, ba, wx, bx, lam, reverse):
    r = jax.nn.sigmoid(_block_diag(u, wa, ba))
    i = jax.nn.sigmoid(_block_diag(u, wx, bx))
    log_a = -LRU_C * r * jax.nn.softplus(-lam)
    a = jnp.exp(log_a)
    b = jnp.sqrt(-jnp.expm1(2.0 * log_a)) * (i * u)
    return _linear_scan(a, b, h0, reverse)


def _rglru_bidir(u_ctx, u_lat, conv_w, conv_b, wa, ba, wx, bx, lam):
    cw = conv_w.astype(F32)
    cb = conv_b.astype(F32)
    v_ctx = _dwconv(u_ctx.astype(F32), cw, RNN_CONV_LEFT) + cb
    v_lat = _dwconv(u_lat.astype(F32), cw, RNN_CONV_LEFT) + cb
    h0 = jnp.zeros((u_ctx.shape[0], D_RNN), F32)
    fwd = (wa[0].astype(F32), ba[0].astype(F32), wx[0].astype(F32), bx[0].astype(F32), lam[0].astype(F32))
    bwd = (wa[1].astype(F32), ba[1].astype(F32), wx[1].astype(F32), bx[1].astype(F32), lam[1].astype(F32))
    hf_ctx = _rglru_scan(v_ctx, h0, *fwd, reverse=False)
    hb_ctx = _rglru_scan(v_ctx, h0, *bwd, reverse=True)
    hf_lat = _rglru_scan(v_lat, hf_ctx[:, -1], *fwd, reverse=False)
    hb_lat = _rglru_scan(v_lat, hb_ctx[:, 0], *bwd, reverse=True)
    return hf_ctx, hb_ctx, hf_lat + hb_lat


def _hyena_filters(n, w1, b1, f1, w2, b2, f2, w3, b3):
    bands = (FILTER_EMB - 1) // 2
    pos = jnp.arange(n, dtype=F32)
    t = pos / max(n - 1, 1)
    ang = (2.0 * math.pi * pos / n)[:, None] * jnp.linspace(1e-4, bands - 1, bands, dtype=F32)[None, :]
    z = jnp.concatenate([t[:, None], jnp.cos(ang), -jnp.sin(ang)], axis=-1)
    hdn = jnp.sin(f1.astype(F32) * (z @ w1.astype(F32) + b1.astype(F32)))
    hdn = jnp.sin(f2.astype(F32) * (hdn @ w2.astype(F32) + b2.astype(F32)))
    k = (hdn @ w3.astype(F32) + b3.astype(F32)).reshape(n, 2, HYENA_ORDER, D_HYENA)
    min_decay = math.log(DECAY_TARGET) / DECAY_SLOW_PCT
    max_decay = math.log(DECAY_TARGET) / DECAY_FAST_PCT
    deltas = jnp.abs(jnp.linspace(min_decay, max_decay, D_HYENA, dtype=F32))
    k = k * jnp.exp(-t[:, None] * deltas[None, :])[:, None, None, :]
    g = jnp.concatenate([k[:, 0], jnp.zeros((1, HYENA_ORDER, D_HYENA), F32), k[:0:-1, 1]], axis=0)
    g = g / jnp.sum(jnp.abs(g), axis=0, keepdims=True)
    return jnp.fft.rfft(g, axis=0)


def _long_conv(u, spec, bias):
    n = u.shape[1]
    y = jnp.fft.irfft(jnp.fft.rfft(u, n=2 * n, axis=1) * spec[None], n=2 * n, axis=1)[:, :n]
    return y + u * bias


def _hyena(u, conv_w, fbias, w1, b1, f1, w2, b2, f2, w3, b3):
    n = u.shape[1]
    z_all = _dwconv(u.astype(F32), conv_w.astype(F32), HYENA_SHORT_W // 2)
    v, *gates = jnp.split(z_all, HYENA_ORDER + 1, axis=-1)
    spec = _hyena_filters(n, w1, b1, f1, w2, b2, f2, w3, b3)
    fb = fbias.astype(F32)
    z = v
    for o in range(HYENA_ORDER):
        z = gates[o] * _long_conv(z, spec[:, o], fb[o])
    return z


def _merge(ya, yb, g_a, g_b, w_out, dt):
    y = jnp.concatenate([_rmsnorm(ya, g_a), _rmsnorm(yb, g_b)], axis=-1).astype(dt)
    return y @ w_out


def _moe(h, w_rg, b_rg, w_re, b_re, w_gate, w_up, w_down):
    shp = h.shape
    t = h.reshape(-1, shp[-1])
    t32 = t.astype(F32)
    g_prob = jax.nn.softmax(t32 @ w_rg.astype(F32) + b_rg.astype(F32), axis=-1)
    g_p, g_i = lax.top_k(g_prob, 1)
    g_onehot = jax.nn.one_hot(g_i[:, 0], N_GROUPS, dtype=F32)
    e_logits = (t32 @ w_re.astype(F32) + b_re.astype(F32)).reshape(-1, N_GROUPS, EXPERTS_PER_GROUP)
    e_prob = jax.nn.softmax(jnp.einsum("tge,tg->te", e_logits, g_onehot), axis=-1)
    e_p, e_i = lax.top_k(e_prob, TOP_K)
    wts = g_p * e_p / jnp.sum(e_p, axis=-1, keepdims=True)
    idx = g_i * EXPERTS_PER_GROUP + e_i
    combine = jnp.einsum("tk,tke->te", wts, jax.nn.one_hot(idx, N_EXPERTS, dtype=F32)).astype(t.dtype)
    out = jnp.zeros_like(t)
    for e in range(N_EXPERTS):
        ye = (jax.nn.silu(t @ w_gate[e]) * (t @ w_up[e])) @ w_down[e]
        out = out + combine[:, e:e + 1] * ye
    return out.reshape(shp)


def setup_inputs(seed: int = 0) -> dict:
    key = jax.random.key(seed)
    ks = iter(jax.random.split(key, 48))
    D = D_MODEL

    def nrm(shape, scale):
        return scale * jax.random.normal(next(ks), shape, F32)

    lam_u = jax.random.uniform(next(ks), (DEPTH, 2, D_RNN), F32, 0.9, 0.999)
    a0 = lam_u ** (1.0 / LRU_C)
    lam = jnp.log(a0) - jnp.log1p(-a0)
    return {
        "x": nrm((BATCH, SEQ, D), 1.0),
        "c": nrm((BATCH, D), 1.0),
        "ctx": nrm((BATCH, CTX_LEN, D), 1.0),
        "c_ctx": nrm((D,), 1.0),
        "w_ada": nrm((DEPTH, D, N_MOD * D), 0.5 * D ** -0.5),
        "b_ada": nrm((DEPTH, N_MOD * D), 0.01),
        "norm1_g": 1.0 + nrm((DEPTH, D), 0.02),
        "w_in": nrm((DEPTH, D, D_IN), D ** -0.5),
        "conv_a_w": nrm((DEPTH, RNN_CONV_W, D_RNN), RNN_CONV_W ** -0.5),
        "conv_a_b": nrm((DEPTH, D_RNN), 0.01),
        "lru_wa": nrm((DEPTH, 2, RNN_HEADS, RNN_HEAD_DIM, RNN_HEAD_DIM), RNN_HEAD_DIM ** -0.5),
        "lru_ba": nrm((DEPTH, 2, D_RNN), 0.01),
        "lru_wx": nrm((DEPTH, 2, RNN_HEADS, RNN_HEAD_DIM, RNN_HEAD_DIM), RNN_HEAD_DIM ** -0.5),
        "lru_bx": nrm((DEPTH, 2, D_RNN), 0.01),
        "lru_lambda": lam,
        "conv_b_w": nrm((DEPTH, HYENA_SHORT_W, (HYENA_ORDER + 1) * D_HYENA), HYENA_SHORT_W ** -0.5),
        "filt_w1": nrm((DEPTH, FILTER_EMB, FILTER_HIDDEN), FILTER_EMB ** -0.5),
        "filt_b1": nrm((DEPTH, FILTER_HIDDEN), 0.1),
        "filt_freq1": 1.0 + nrm((DEPTH, FILTER_HIDDEN), 0.02),
        "filt_w2": nrm((DEPTH, FILTER_HIDDEN, FILTER_HIDDEN), FILTER_HIDDEN ** -0.5),
        "filt_b2": nrm((DEPTH, FILTER_HIDDEN), 0.1),
        "filt_freq2": 1.0 + nrm((DEPTH, FILTER_HIDDEN), 0.02),
        "filt_w3": nrm((DEPTH, FILTER_HIDDEN, 2 * HYENA_ORDER * D_HYENA), FILTER_HIDDEN ** -0.5),
        "filt_b3": nrm((DEPTH, 2 * HYENA_ORDER * D_HYENA), 0.01),
        "filt_bias": nrm((DEPTH, HYENA_ORDER, D_HYENA), 0.1),
        "out_norm_a": 1.0 + nrm((DEPTH, D_RNN), 0.02),
        "out_norm_b": 1.0 + nrm((DEPTH, D_HYENA), 0.02),
        "w_out": nrm((DEPTH, D_MIX, D), D_MIX ** -0.5),
        "norm2_g": 1.0 + nrm((DEPTH, D), 0.02),
        "w_rg": nrm((DEPTH, D, N_GROUPS), D ** -0.5),
        "b_rg": nrm((DEPTH, N_GROUPS), 0.01),
        "w_re": nrm((DEPTH, D, N_EXPERTS), D ** -0.5),
        "b_re": nrm((DEPTH, N_EXPERTS), 0.01),
        "w_gate": nrm((DEPTH, N_EXPERTS, D, D_EXPERT), D ** -0.5),
        "w_up": nrm((DEPTH, N_EXPERTS, D, D_EXPERT), D ** -0.5),
        "w_down": nrm((DEPTH, N_EXPERTS, D_EXPERT, D), D_EXPERT ** -0.5),
        "final_g": 1.0 + nrm((D,), 0.02),
    }


def reference(x, c, ctx, c_ctx, w_ada, b_ada, norm1_g, w_in, conv_a_w, conv_a_b, lru_wa, lru_ba,
              lru_wx, lru_bx, lru_lambda, conv_b_w, filt_w1, filt_b1, filt_freq1, filt_w2, filt_b2,
              filt_freq2, filt_w3, filt_b3, filt_bias, out_norm_a, out_norm_b, w_out, norm2_g,
              w_rg, b_rg, w_re, b_re, w_gate, w_up, w_down, final_g):
    dt = x.dtype
    n_lat = x.shape[1]
    rows = n_lat // GRID_W
    x = x + _sincos_2d(rows, GRID_W, x.shape[-1]).astype(dt)[None]
    xc = ctx
    for l in range(DEPTH):
        last = l == DEPTH - 1
        mod = jax.nn.silu(c) @ w_ada[l] + b_ada[l]
        mod_c = jax.nn.silu(c_ctx) @ w_ada[l] + b_ada[l]
        sh1, sc1, g1, sh2, sc2, g2 = jnp.split(mod[:, None, :], N_MOD, axis=-1)
        csh1, csc1, cg1, csh2, csc2, cg2 = jnp.split(mod_c, N_MOD, axis=-1)

        p = _modulate(_rmsnorm(x, norm1_g[l]), sh1, sc1) @ w_in[l]
        pc = _modulate(_rmsnorm(xc, norm1_g[l]), csh1, csc1) @ w_in[l]
        filt = (filt_w1[l], filt_b1[l], filt_freq1[l], filt_w2[l], filt_b2[l], filt_freq2[l],
                filt_w3[l], filt_b3[l])

        hf_ctx, hb_ctx, rnn_lat = _rglru_bidir(pc[..., D_RNN:2 * D_RNN], p[..., D_RNN:2 * D_RNN],
                                               conv_a_w[l], conv_a_b[l], lru_wa[l], lru_ba[l],
                                               lru_wx[l], lru_bx[l], lru_lambda[l])
        ya = jax.nn.gelu(p[..., :D_RNN].astype(F32), approximate=True) * rnn_lat
        yb = _hyena(p[..., 2 * D_RNN:], conv_b_w[l], filt_bias[l], *filt)
        y = _merge(ya, yb, out_norm_a[l], out_norm_b[l], w_out[l], dt)

        if not last:
            yac = jax.nn.gelu(pc[..., :D_RNN].astype(F32), approximate=True) * (hf_ctx + hb_ctx)
            ybc = _hyena(pc[..., 2 * D_RNN:], conv_b_w[l], filt_bias[l], *filt)
            xc = xc + cg1 * _merge(yac, ybc, out_norm_a[l], out_norm_b[l], w_out[l], dt)
            hc2 = _modulate(_rmsnorm(xc, norm2_g[l]), csh2, csc2)
            xc = xc + cg2 * _moe(hc2, w_rg[l], b_rg[l], w_re[l], b_re[l], w_gate[l], w_up[l], w_down[l])

        x = x + g1 * y
        h2 = _modulate(_rmsnorm(x, norm2_g[l]), sh2, sc2)
        x = x + g2 * _moe(h2, w_rg[l], b_rg[l], w_re[l], b_re[l], w_gate[l], w_up[l], w_down[l])
    return _rmsnorm(x, final_g)
```

```python
import math
from contextlib import ExitStack
import numpy as np
import concourse.bass as bass
import concourse.mybir as mybir
from concourse.bass_utils import run_bass_kernel_spmd

F32 = mybir.dt.float32
BF16 = mybir.dt.bfloat16
I32 = mybir.dt.int32
AF = mybir.ActivationFunctionType
ALU = mybir.AluOpType
AX = mybir.AxisListType

D = 1024
T = 2048
TC = 256
NT = 16
EPS = 1e-6
NE = 32
DE = 512
TWO_PI = 2.0 * math.pi
VAR = ''

R_C, R_CC, R_BADA, R_N1, R_N2, R_CAW, R_CAB, R_BA, R_BX, R_LAM, R_CBW, R_ONA, R_ONB = (
    0, 8, 16, 64, 72, 80, 96, 100, 108, 116, 124, 160, 164)
NR = 168


class FW:
    ENG = ("pe", "act", "dve", "pool", "sp")
    SEG = 16000
    NDMA = 28
    NHW = 20

    def __init__(self, nc, es):
        self.nc = nc
        self.es = es
        self.ops = {e: [] for e in self.ENG}
        self.lastw = {}
        self.readers = {}
        self.waited = {e: {} for e in self.ENG}
        self.dma_cnt = [0] * self.NDMA
        self.dma_rr = 0
        self.dma_rr_sw = 0
        self.enabled = True

    def op(self, eng, fn, reads=(), writes=(), dma=False):
        if not self.enabled:
            return None
        pr = [k for k in reads if isinstance(k, str) and k[:2] == 'ps' and k[2:].isdigit()]
        idx = len(self.ops[eng])
        need = {}

        def add(tok):
            if tok[0] == 'c':
                if tok[1] == eng and eng == 'pe':
                    return
                key = ('c', tok[1])
            else:
                key = ('d', tok[1])
            if need.get(key, -1) < tok[2]:
                need[key] = tok[2]

        for k in reads:
            t = self.lastw.get(k)
            if t is not None:
                add(t)
        for k in pr:
            for r in self.readers.get(k, ()):
                if r[0] == 'c' and r[1] != eng:
                    add(r)
        for k in writes:
            t = self.lastw.get(k)
            if t is not None:
                add(t)
            for r in self.readers.get(k, ()):
                add(r)
        waits = []
        wd = self.waited[eng]
        for key, val in need.items():
            if wd.get(key, -1) >= val:
                continue
            wd[key] = val
            waits.append((key[0], key[1], val))
        if dma:
            if eng == 'pool':
                s = self.NHW + self.dma_rr_sw
                self.dma_rr_sw = (self.dma_rr_sw + 1) % (self.NDMA - self.NHW)
            else:
                s = self.dma_rr
                self.dma_rr = (s + 1) % self.NHW
            prev = self.dma_cnt[s]
            if prev > 0 and wd.get(('d', s), -1) < prev:
                wd[('d', s)] = prev
                waits.append(('d', s, prev))
            self.dma_cnt[s] = prev + 16
            tok = ('d', s, prev + 16)
        else:
            tok = ('c', eng, idx)
        self.ops[eng].append(dict(fn=fn, waits=waits, tok=tok))
        for k in writes:
            self.lastw[k] = tok
            self.readers[k] = []
        for k in reads:
            self.readers.setdefault(k, []).append(tok)
        return tok

    def barrier(self):
        if not self.enabled:
            return
        toks = []
        for e in self.ENG:
            n = len(self.ops[e])
            for i in range(n - 1, -1, -1):
                o = self.ops[e][i]
                if o['tok'][0] == 'c' and o['fn'] is not None:
                    toks.append(('c', e, i))
                    break
        for s in range(self.NDMA):
            if self.dma_cnt[s] > 0:
                toks.append(('d', s, self.dma_cnt[s]))
        for e in self.ENG:
            waits = []
            wd = self.waited[e]
            for t in toks:
                if t[0] == 'c' and t[1] == e and e == 'pe':
                    continue
                key = (t[0], t[1])
                if wd.get(key, -1) >= t[2]:
                    continue
                wd[key] = t[2]
                waits.append(t)
            if waits:
                self.ops[e].append(dict(fn=None, waits=waits, tok=('n', e, len(self.ops[e]))))

    def emit(self):
        nc = self.nc
        sig = {e: set() for e in self.ENG}
        for e in self.ENG:
            for o in self.ops[e]:
                for w in o['waits']:
                    if w[0] == 'c':
                        sig[w[1]].add(w[2])
        tick = {}
        nsem = {}
        for e in self.ENG:
            for r, i in enumerate(sorted(sig[e])):
                tick[(e, i)] = r + 1
            nsem[e] = (len(sig[e]) + self.SEG - 1) // self.SEG
        esem = {e: [self.es.enter_context(nc.semaphore("s_%s_%d" % (e, j))) for j in range(nsem[e])]
                for e in self.ENG}
        dsem = [self.es.enter_context(nc.semaphore("s_dma_%d" % j)) for j in range(self.NDMA)]
        SEG = self.SEG

        def semval(e, i):
            t = tick[(e, i)] - 1
            return esem[e][t // SEG], (t % SEG) + 1

        def run(e, eng):
            for i, o in enumerate(self.ops[e]):
                for w in o['waits']:
                    if w[0] == 'c':
                        s, v = semval(w[1], w[2])
                    else:
                        s, v = dsem[w[1]], w[2]
                    eng.wait_ge(s, v)
                if o['fn'] is None:
                    continue
                ins = o['fn'](eng)
                if o['tok'][0] == 'd':
                    ins.then_inc(dsem[o['tok'][1]], 16)
                elif i in sig[e]:
                    s, v = semval(e, i)
                    ins.then_inc(s, 1)

        with nc.Block() as block:
            @block.tensor
            def _(eng):
                run('pe', eng)

            @block.scalar
            def _(eng):
                run('act', eng)

            @block.vector
            def _(eng):
                run('dve', eng)

            @block.gpsimd
            def _(eng):
                run('pool', eng)

            @block.sync
            def _(eng):
                run('sp', eng)
        return {e: len(self.ops[e]) for e in self.ENG}


class Alloc:
    def __init__(self, arena, start, end):
        self.arena = arena
        self.off = start
        self.end = end

    def _take(self, nwords):
        self.off = (self.off + 15) // 16 * 16
        o = self.off
        self.off += nwords
        assert self.off <= self.end, ("arena overflow", self.off, self.end)
        return o

    def _shape(self, ap, shape):
        if len(shape) == 2:
            return ap
        if len(shape) == 3:
            return ap.rearrange("p (a b) -> p a b", a=shape[1])
        if len(shape) == 4:
            return ap.rearrange("p (a b c) -> p a b c", a=shape[1], b=shape[2])
        raise ValueError(shape)

    def f32(self, shape):
        n = int(np.prod(shape[1:]))
        o = self._take(n)
        return self._shape(self.arena[0:shape[0], o:o + n], shape)

    def i32(self, shape):
        n = int(np.prod(shape[1:]))
        o = self._take(n)
        return self._shape(self.arena[0:shape[0], o:o + n].bitcast(I32), shape)

    def bf16(self, shape):
        n = int(np.prod(shape[1:]))
        w = (n + 1) // 2
        o = self._take(w)
        ap = self.arena[0:shape[0], o:o + w].bitcast(BF16)
        if 2 * w != n:
            ap = ap[:, 0:n]
        return self._shape(ap, shape)


def build_program(debug=False, stop=None):
    nc = bass.Bass("TRN2", target_bir_lowering=False)

    def din(name, shape, dt=F32):
        return nc.dram_tensor(name, list(shape), dt, kind="ExternalInput").ap()

    skind = "ExternalOutput" if debug else "Internal"

    def dscr(name, shape, dt):
        return nc.dram_tensor(name, list(shape), dt, kind=skind).ap()

    x_d = din("x", [T, D])
    ctx_d = din("ctx", [TC, D])
    rows_d = din("rows", [NR, 128])
    w_ada_d = din("w_ada", [D, 6 * D])
    b_ada_d = din("b_ada", [1, 6 * D])
    w_in_d = din("w_in", [D, 2560])
    lru_wa_d = din("lru_wa", [16, 64, 64])
    lru_wx_d = din("lru_wx", [16, 64, 64])
    filt_w1_d = din("filt_w1", [33, 64])
    filt_cols_d = din("filt_cols", [64, 4])
    filt_w2_d = din("filt_w2", [64, 64])
    filt_w3_d = din("filt_w3", [64, 2048])
    filt_b3_d = din("filt_b3", [1, 2048])
    filt_bias_d = din("filt_bias", [1, 1024])
    w_out_d = din("w_out", [D, D])
    w_r_d = din("w_r", [D, 36])
    b_r_d = din("b_r", [1, 36])
    w_gate_d = din("w_gate", [NE, D, DE])
    w_up_d = din("w_up", [NE, D, DE])
    w_down_d = din("w_down", [NE, DE, D])
    final_g_d = din("final_g", [1, D])
    out_d = nc.dram_tensor("out", [T, D], F32, kind="ExternalOutput").ap()

    xpe_s = dscr("xpe_s", [NT, 128, D], F32)
    ya_s = dscr("ya_s", [4, 128, T], BF16)
    yb_s = dscr("yb_s", [4, 128, T], BF16)
    hy_s = dscr("hy_s", [3, 128, NT, 512], BF16)
    dftc_s = dscr("dftc_s", [16, 128, NT, 128], BF16)
    dfts_s = dscr("dfts_s", [16, 128, NT, 128], BF16)
    dbg_s = dscr("dbg_s", [8, 128, 2320], F32) if debug else None

    es = ExitStack()
    with es:
        fw = FW(nc, es)
        NW = 53200
        arena = es.enter_context(nc.sbuf_tensor("arena", [128, NW], F32))
        psum = es.enter_context(nc.psum_tensor("psum", [128, 8, 512], F32))
        st = dict(psrr=0)

        st['nb'] = 8

        def bank():
            b = st['psrr'] % st['nb']
            st['psrr'] = (b + 1) % st['nb']
            return b

        def chk(name):
            if stop == name:
                fw.barrier()
                fw.enabled = False

        def mm(out, lhsT, rhs, start, stop, r, w):
            fw.op('pe', lambda e: e.matmul(out, lhsT, rhs, start=start, stop=stop), r, w)

        def tr(out, in_, ident, r, w):
            fw.op('pe', lambda e: e.transpose(out, in_, ident), r, w)

        def act(out, in_, func, r, w, scale=None, bias=None, accum=None):
            kw = {}
            if scale is not None:
                kw['scale'] = scale
            if bias is not None:
                kw['bias'] = bias
            if accum is not None:
                kw['accum_out'] = accum
            fw.op('act', lambda e: e.activation(out=out, in_=in_, func=func, **kw), r, w)

        def ts(eng, out, in0, s1, s2, op0, op1, r, w):
            if s2 is None:
                fw.op(eng, lambda e: e.tensor_scalar(out=out, in0=in0, scalar1=s1, scalar2=None, op0=op0), r, w)
            else:
                fw.op(eng, lambda e: e.tensor_scalar(out=out, in0=in0, scalar1=s1, scalar2=s2, op0=op0, op1=op1), r, w)

        def stt(eng, out, in0, scalar, in1, op0, op1, r, w):
            fw.op(eng, lambda e: e.scalar_tensor_tensor(out=out, in0=in0, scalar=scalar, in1=in1, op0=op0, op1=op1), r, w)

        def tt(eng, out, in0, in1, op, r, w):
            fw.op(eng, lambda e: e.tensor_tensor(out=out, in0=in0, in1=in1, op=op), r, w)

        def cp(eng, out, in_, r, w):
            if eng == 'act':
                fw.op(eng, lambda e: e.activation(out=out, in_=in_, func=AF.Copy), r, w)
            else:
                fw.op(eng, lambda e: e.tensor_copy(out=out, in_=in_), r, w)

        def mset(eng, out, val, w):
            fw.op(eng, lambda e: e.memset(out, val), (), w)

        def dma(q, out, in_, r, w):
            fw.op(q, lambda e: e.dma_start(out=out, in_=in_), r, w, dma=True)

        def iota(out, pattern, base, cm, w):
            fw.op('pool', lambda e: e.iota(out, pattern, base=base, channel_multiplier=cm), (), w)

        P = Alloc(arena, 0, 4200)
        ident_f = P.f32([128, 128])
        ident_b = P.bf16([128, 128])
        ones_f = P.f32([128, 128])
        cst = P.f32([128, 8])
        cols = P.f32([128, NR])
        modc = P.f32([128, 32, 2])
        scl1 = P.f32([128, 8, 2])
        scl2 = P.f32([128, 8])
        g_rep = P.f32([128, 4, 512])
        ssa = P.f32([128, 4, 16])
        rstd_a = P.f32([128, 16])
        rstd_b = P.f32([128, 16])
        misc = P.f32([128, 64])
        C_NEGPI, C_ONE, C_EPS, C_ZERO = 0, 1, 2, 3

        A = Alloc(arena, P.end, NW)
        W = [A.f32([128, 2320]) for _ in range(8)]
        Wx = A.f32([128, 2320])
        amark = A.off
        E = A.f32([64, 512])
        selr = A.f32([64, 16, 128])
        selc = A.f32([64, 128])
        pec = A.f32([128, 512])
        NXT = 4
        xt = [A.f32([128, 1024]) for _ in range(NXT)]
        junk = A.bf16([128, 1024])
        Al = Alloc(arena, amark, A.off)
        W.append(Al.f32([128, 2320]))
        W.append(Al.f32([128, 2320]))
        W.append(Wx)
        A.off = (A.off + 15) // 16 * 16
        mark0 = A.off
        hT = A.bf16([128, 8, T])
        hcT = A.bf16([128, 8, TC])
        wib = [A.bf16([128, 8, 128]) for _ in range(3)]
        Wbd = A.f32([128, 16, 128])
        mark1 = A.off
        Wbdb = A.bf16([128, 16, 128])
        Vbp = [A.bf16([128, 2320]) for _ in range(2)]
        hcb = [A.bf16([128, T]) for _ in range(2)]
        hyst = arena[:, (P.end + 15) // 16 * 16 + 6 * 2320:(P.end + 15) // 16 * 16 + 6 * 2320 + 4096].bitcast(BF16).rearrange("p (a b) -> p a b", a=16)
        yast = hcb[0]
        halfb = A.f32([128, 16])
        spc = A.f32([128, 8, 4])
        tcol = A.f32([128, 16])
        tcol_i = A.i32([128, 16])
        ss1 = misc[:, 0:18]
        rs1 = misc[:, 18:36]
        rstd1 = misc[:, 36:54]

        def bank01():
            b = st.get('b01', 0)
            st['b01'] = 1 - b
            return b

        def pair():
            p = st.get('pair', 0)
            st['pair'] = (p + 1) % 3
            return 2 + 2 * p

        A0 = Alloc(arena, mark0, mark1)
        idi = A0.i32([128, 128])
        iota(idi, [[1, 128]], 0, -1, ['idi'])
        cp('dve', ident_f, idi, ['idi'], ['ident_f'])
        fw.op('dve', lambda e: e.tensor_single_scalar(out=ident_f, in_=ident_f, scalar=0.0, op=ALU.is_equal),
              ['ident_f'], ['ident_f'])
        cp('dve', ident_b, ident_f, ['ident_f'], ['ident_b'])
        mset('dve', ones_f, 1.0, ['ones_f'])
        mset('dve', cst[:, C_NEGPI:C_NEGPI + 1], -math.pi, ['cst'])
        mset('dve', cst[:, C_ONE:C_ONE + 1], 1.0, ['cst'])
        mset('dve', cst[:, C_EPS:C_EPS + 1], EPS, ['cst'])
        mset('dve', cst[:, C_ZERO:C_ZERO + 1], 0.0, ['cst'])
        negpi = cst[:, C_NEGPI:C_NEGPI + 1]
        one_c = cst[:, C_ONE:C_ONE + 1]
        eps_c = cst[:, C_EPS:C_EPS + 1]

        def sin_reduce(out, in_, np_, p0, off_turns, tA, tB, tI, r, w, tkey):
            ts('dve', tA, in_, 1.0 / TWO_PI, 16.5 + off_turns, ALU.mult, ALU.add, r, [tkey + 'A'])
            cp('dve', tI, tA, [tkey + 'A'], [tkey + 'I'])
            cp('dve', tB, tI, [tkey + 'I'], [tkey + 'B'])
            tt('dve', tA, tA, tB, ALU.subtract, [tkey + 'A', tkey + 'B'], [tkey + 'A'])
            stt('dve', tA, tA, 0.0, tA, ALU.is_lt, ALU.add, [tkey + 'A'], [tkey + 'A'])
            p0 = p0 or 0
            act(out, tA, AF.Sin, [tkey + 'A', 'cst'], w, scale=TWO_PI, bias=negpi[p0:p0 + np_, :])

        rows_sb = A0.f32([128, 2, 128])
        dma('sp', rows_sb[:, 0, :], rows_d[0:128, :], [], ['rows0'])
        dma('sp', rows_sb[0:NR - 128, 1, :], rows_d[128:NR, :], [], ['rows1'])
        b0 = bank()
        tr(psum[:, b0, 0:128], rows_sb[:, 0, :], ident_f, ['rows0', 'ident_f'], ['ps%d' % b0])
        tr(psum[:, b0, 128:NR], rows_sb[0:NR - 128, 1, :], ident_f[0:NR - 128, 0:NR - 128],
           ['rows1', 'ident_f'], ['ps%d' % b0])
        cp('dve', cols, psum[:, b0, 0:NR], ['ps%d' % b0], ['cols'])

        chk('const')
        sc = A0.f32([128, 8, 2])
        rep = A0.f32([128, 8, 128])
        bada_row = A0.f32([1, 2048])
        wa_buf = [A0.f32([128, 8, 256]) for _ in range(4)]
        act(sc[:, :, 0], cols[:, R_C:R_C + 8], AF.Silu, ['cols'], ['sc'])
        act(sc[:, :, 1], cols[:, R_CC:R_CC + 8], AF.Silu, ['cols', 'sc'], ['sc'])
        for k in range(8):
            ts('dve', rep[:, k, :], ones_f, sc[:, k, 0:1], None, ALU.mult, None, ['ones_f', 'sc'], ['rep'])
        dma('sp', bada_row[:, 0:1024], b_ada_d[:, 2048:3072], [], ['bada_row'])
        dma('sp', bada_row[:, 1024:2048], b_ada_d[:, 5120:6144], [], ['bada_row'])
        CB = [0, 1, 2, 3, 6, 7, 8, 9]
        w_ada_v = w_ada_d.rearrange("(k p) n -> p k n", p=128)

        def mod_load(hb):
            s = hb % 4
            dma('sp', wa_buf[s], w_ada_v[:, :, 256 * hb:256 * hb + 256], [], ['wa%d' % s])

        def mod_block(hb):
            s = hb % 4
            blk = hb // 2
            hf = hb % 2
            if blk in CB:
                jb = CB.index(blk)
                for c2 in range(2):
                    cc = 2 * hf + c2
                    b = bank()
                    for k in range(8):
                        mm(psum[:, b, 0:2], wa_buf[s][:, k, 128 * c2:128 * c2 + 128], sc[:, k, :],
                           k == 0, k == 7, ['wa%d' % s, 'sc'], ['ps%d' % b])
                    j = jb * 4 + cc
                    piece = R_BADA + blk * 4 + cc
                    ts('dve', modc[:, j, :], psum[:, b, 0:2], cols[:, piece:piece + 1], None, ALU.add, None,
                       ['ps%d' % b, 'cols'], ['modc'])
            else:
                gi = {4: 0, 5: 1, 10: 2, 11: 3}[blk]
                b = bank()
                for k in range(8):
                    mm(psum[:, b, 0:256], rep[:, k, :], wa_buf[s][:, k, :], k == 0, False,
                       ['wa%d' % s, 'rep'], ['ps%d' % b])
                mm(psum[:, b, 0:256], ones_f[0:1, :], bada_row[0:1, 512 * gi + 256 * hf:512 * gi + 256 * hf + 256],
                   False, True, ['ones_f', 'bada_row'], ['ps%d' % b])
                cp('act', g_rep[:, gi, 256 * hf:256 * hf + 256], psum[:, b, 0:256], ['ps%d' % b], ['g_rep'])

        kvec_i = W[1][:, 0:2048].bitcast(I32)
        kvec = W[0][:, 0:2048]
        iota(kvec_i, [[2, 2048]], 1, 0, ['W1'])
        cp('dve', kvec, kvec_i, ['W1'], ['W0'])
        iota(tcol_i, [[256, 16]], 1, 2, ['tcol_i'])
        cp('dve', tcol, tcol_i, ['tcol_i'], ['tcol'])
        PFi = W[1][:, 0:2048].bitcast(I32)
        MSi = W[2][:, 0:2048].bitcast(I32)
        PCi = W[3][:, 0:2048].bitcast(I32)
        PF3 = PFi.rearrange("p (i q) -> p i q", i=16)
        tcb = tcol.unsqueeze(2).to_broadcast([128, 16, 128])

        def dft_iter(j):
            kb_ = kvec[:, 128 * j:128 * j + 128].unsqueeze(1).to_broadcast([128, 16, 128])
            tt('dve', PF3, tcb, kb_, ALU.mult, ['W0', 'tcol'], ['W1'])
            fw.op('dve', lambda e: e.tensor_single_scalar(out=MSi, in_=PFi, scalar=16383, op=ALU.bitwise_and),
                  ['W1'], ['W2'])
            act(hcb[0], MSi, AF.Sin, ['W2', 'cst'], ['hcb0'], scale=TWO_PI / 16384.0, bias=negpi)
            dma('sp', dfts_s[j].rearrange("p i q -> p (i q)"), hcb[0], ['hcb0'], [('dfts', j)])
            fw.op('dve', lambda e: e.tensor_single_scalar(out=PCi, in_=PFi, scalar=4096, op=ALU.add),
                  ['W1'], ['W3'])
            fw.op('dve', lambda e: e.tensor_single_scalar(out=PCi, in_=PCi, scalar=16383, op=ALU.bitwise_and),
                  ['W3'], ['W3'])
            act(hcb[1], PCi, AF.Sin, ['W3', 'cst'], ['hcb1'], scale=TWO_PI / 16384.0, bias=negpi)
            dma('sp', dftc_s[j].rearrange("p i q -> p (i q)"), hcb[1], ['hcb1'], [('dftc', j)])

        for hb in range(3):
            mod_load(hb)
        for hb in range(24):
            if hb + 3 < 24:
                mod_load(hb + 3)
            mod_block(hb)
            if hb % 3 != 2:
                dft_iter(hb - hb // 3)
        for v in range(2):
            stt('dve', scl1[:, :, v], modc[:, 8:16, v], 1.0, cols[:, R_N1:R_N1 + 8], ALU.add, ALU.mult,
                ['modc', 'cols'], ['scl1'])
        stt('dve', scl2, modc[:, 24:32, 0], 1.0, cols[:, R_N2:R_N2 + 8], ALU.add, ALU.mult,
            ['modc', 'cols'], ['scl2'])
        fw.barrier()
        chk('mod')

        chk('dft')
        jrow_i = W[5][0:64, 0:256].bitcast(I32)
        jrow_f = W[4][0:64, 0:256]
        omega = W[4][0:64, 256:512]
        ncol_i = W[5][0:64, 256:257].bitcast(I32)
        ncol_f = W[4][0:64, 512:513]
        ang = W[4][0:64, 768:1024]
        iota(jrow_i, [[1, 256]], 0, 0, ['W5'])
        cp('dve', jrow_f, jrow_i, ['W5'], ['W4'])
        act(omega, jrow_f, AF.Exp, ['W4'], ['W4'], scale=-math.log(10000.0) / 256.0)
        iota(ncol_i, [[0, 1]], 0, 1, ['W5'])
        cp('dve', ncol_f, ncol_i, ['W5'], ['W4'])
        ts('dve', ang, omega, ncol_f, None, ALU.mult, None, ['W4'], ['W4'])
        tA = W[4][0:64, 1024:1280]
        tB = W[4][0:64, 1280:1536]
        tI = W[5][0:64, 512:768].bitcast(I32)
        sin_reduce(E[:, 0:256], ang, 64, None, 0.0, tA, tB, tI, ['W4'], ['E'], 'srt')
        sin_reduce(E[:, 256:512], ang, 64, None, 0.25, tA, tB, tI, ['W4'], ['E'], 'srt')
        selc_i = W[5][0:64, 1024:1152].bitcast(I32)
        iota(selc_i.rearrange("p (a b) -> p a b", a=2), [[0, 2], [1, 64]], 0, -1, ['W5'])
        cp('dve', selc, selc_i, ['W5'], ['selc'])
        fw.op('dve', lambda e: e.tensor_single_scalar(out=selc, in_=selc, scalar=0.0, op=ALU.is_equal),
              ['selc'], ['selc'])
        selr_i = W[3][0:64, 0:2048].bitcast(I32)
        iota(selr_i.rearrange("p (a b c) -> p a b c", a=16, b=2), [[2, 16], [1, 2], [0, 64]], 0, -1, ['W3'])
        selr_flat = selr.rearrange("p a b -> p (a b)")
        cp('dve', selr_flat, selr_i, ['W3'], ['selr'])
        fw.op('dve', lambda e: e.tensor_single_scalar(out=selr_flat, in_=selr_flat, scalar=0.0, op=ALU.is_equal),
              ['selr'], ['selr'])
        b = bank01()
        mm(psum[:, b, :], selc, E, True, True, ['selc', 'E'], ['ps%d' % b])
        cp('act', pec, psum[:, b, :], ['ps%d' % b], ['pec'])
        mset('dve', ss1, 0.0, ['ss1'])

        chk('pe')
        mset('dve', Wbd, 0.0, ['Wbd'])
        for dd in range(2):
            for g, src in enumerate((lru_wa_d, lru_wx_d)):
                for c in range(4):
                    slot = (dd * 2 + g) * 4 + c
                    dma('pool', Wbd[0:64, slot, 0:64], src[dd * 8 + 2 * c], ['Wbd'], [('Wbdd', slot)])
                    dma('pool', Wbd[64:128, slot, 64:128], src[dd * 8 + 2 * c + 1], ['Wbd'], [('Wbdd', slot)])
        ts('dve', halfb[:, 0:16], cols[:, R_BA:R_BA + 16], 0.5, None, ALU.mult, None, ['cols'], ['halfb'])
        lamc = cols[:, R_LAM:R_LAM + 8]
        stt('dve', spc[:, :, 2], lamc, -1.0, lamc, ALU.mult, ALU.max, ['cols'], ['spc'])
        act(spc[:, :, 2], spc[:, :, 2], AF.Exp, ['spc'], ['spc'], scale=-1.0)
        act(spc[:, :, 2], spc[:, :, 2], AF.Ln, ['spc', 'cst'], ['spc'], scale=1.0, bias=one_c)
        ts('dve', spc[:, :, 3], lamc, -1.0, 0.0, ALU.mult, ALU.max, ['cols', 'spc'], ['spc'])
        tt('dve', spc[:, :, 2], spc[:, :, 2], spc[:, :, 3], ALU.add, ['spc'], ['spc'])
        ts('dve', spc[:, :, 0], spc[:, :, 2], -4.0, None, ALU.mult, None, ['spc'], ['spc'])
        ts('dve', spc[:, :, 1], spc[:, :, 2], -8.0, None, ALU.mult, None, ['spc'], ['spc'])

        def p1_stage1(ii):
            s = ii % NXT
            lat = ii < NT
            xk = 'xt%d' % s
            src = x_d[128 * ii:128 * ii + 128, :] if lat else ctx_d[128 * (ii - NT):128 * (ii - NT) + 128, :]
            dma('sp', xt[s], src, [], [xk])
            if lat:
                b = bank01()
                mm(psum[:, b, :], selr[:, ii, :], E, True, True, ['selr', 'E'], ['ps%d' % b])
                tt('dve', xt[s][:, 0:512], xt[s][:, 0:512], psum[:, b, :], ALU.add, [xk, 'ps%d' % b], [xk])
                tt('dve', xt[s][:, 512:1024], xt[s][:, 512:1024], pec, ALU.add, [xk, 'pec'], [xk])
                dma('sp', xpe_s[ii], xt[s], [xk], [('xpe', ii)])
            act(junk, xt[s], AF.Square, [xk], ['junk', 'ss1'], accum=ss1[:, ii:ii + 1])
            act(rs1[:, ii:ii + 1], ss1[:, ii:ii + 1], AF.Sqrt, ['ss1', 'cst'], ['rs1'], scale=1.0 / D, bias=eps_c)
            fw.op('dve', lambda e, ii=ii: e.reciprocal(out=rstd1[:, ii:ii + 1], in_=rs1[:, ii:ii + 1]),
                  ['rs1'], ['rstd1'])
            ts('dve', xt[s], xt[s], rstd1[:, ii:ii + 1], None, ALU.mult, None, [xk, 'rstd1'], [xk])

        def p1_stage2(ii):
            s = ii % NXT
            lat = ii < NT
            v = 0 if lat else 1
            xk = 'xt%d' % s
            pb = pair()
            pk = ['ps%d' % pb, 'ps%d' % (pb + 1)]
            psT = psum[:, pb:pb + 2, :].rearrange("p a (b c) -> p (a b) c", c=128)
            for k in range(8):
                tr(psT[:, k, :], xt[s][:, 128 * k:128 * k + 128], ident_f, [xk, 'ident_f'], pk)
            for k in range(8):
                if lat:
                    dest = hT[:, k, 128 * ii:128 * ii + 128]
                else:
                    dest = hcT[:, k, 128 * (ii - NT):128 * (ii - NT) + 128]
                if k < 4:
                    act(dest, psT[:, k, :], AF.Identity, [pk[0], 'scl1', 'modc'], [('hT', ii)],
                        scale=scl1[:, k, v:v + 1], bias=modc[:, k, v:v + 1])
                else:
                    ts('dve', dest, psT[:, k, :], scl1[:, k, v:v + 1], modc[:, k, v:v + 1], ALU.mult, ALU.add,
                       [pk[1], 'scl1', 'modc'], [('hT', ii)])

        p1_stage1(0)
        p1_stage1(1)
        p1_stage1(2)
        for ii in range(NT + 2):
            if ii + 3 < NT + 2:
                p1_stage1(ii + 3)
            p1_stage2(ii)

        chk('p1')
        w_in_v = w_in_d.rearrange("(k p) n -> p k n", p=128)
        order = []
        for c in range(4):
            order += [c, 4 + c]
        order += list(range(8, 20))
        st['wslot'] = {}

        def load_win(pos):
            if pos >= len(order):
                return
            cc = order[pos]
            s = pos % 3
            st['wslot'][cc] = s
            dma('pool', wib[s], w_in_v[:, :, 128 * cc:128 * cc + 128], [], ['wib%d' % s])

        load_win(0)
        load_win(1)
        hkeys = [[('hT', 4 * n + j) for j in range(4)] for n in range(4)]
        ckeys = [('hT', NT), ('hT', NT + 1)]

        def proj(cc, evac, evac_ctx=None):
            s = st['wslot'][cc]
            wk = 'wib%d' % s
            for n in range(4):
                b = bank()
                for k in range(8):
                    mm(psum[:, b, :], wib[s][:, k, :], hT[:, k, 512 * n:512 * n + 512], k == 0, k == 7,
                       [wk] + hkeys[n], ['ps%d' % b])
                evac(n, b)
            if evac_ctx is not None:
                b = bank()
                for k in range(8):
                    mm(psum[:, b, 0:TC], wib[s][:, k, :], hcT[:, k, :], k == 0, k == 7, [wk] + ckeys, ['ps%d' % b])
                evac_ctx(b)

        st['pos'] = 0
        NV = 2307
        UXp, Vp, GAp = [W[0], W[8]], [W[1], W[9]], [W[2], W[10]]
        UXk, Vk, GAk = ['W0', 'W8'], ['W1', 'W9'], ['W2', 'W10']
        blocks = [(0, 512), (512, 1024), (1024, 1536), (1536, 2048), (2048, NV)]

        def rnn_stageA(c):
            par = c % 2
            U, V, GA, Vb = UXp[par], Vp[par], GAp[par], Vbp[par]
            uk, vk, gk, vbk = UXk[par], Vk[par], GAk[par], 'Vb%d' % par
            load_win(st['pos'] + 2)

            def ev_ga(n, b):
                act(GA[:, 512 * n:512 * n + 512], psum[:, b, :], AF.Gelu_apprx_tanh, ['ps%d' % b], [gk])
            proj(c, ev_ga)
            st['pos'] += 1
            load_win(st['pos'] + 2)
            mset('dve', U[:, 0:2], 0.0, [uk])
            mset('dve', U[:, 258:261], 0.0, [uk])
            mset('dve', U[:, 2309:2310], 0.0, [uk])

            def ev_u(n, b):
                eng = 'dve' if n % 2 == 0 else 'act'
                cp(eng, U[:, 261 + 512 * n:261 + 512 * n + 512], psum[:, b, :], ['ps%d' % b], [uk])

            def ev_uc(b):
                cp('dve', U[:, 2:2 + TC], psum[:, b, 0:TC], ['ps%d' % b], [uk])
            proj(4 + c, ev_u, ev_uc)
            st['pos'] += 1
            wc = [cols[:, R_CAW + 4 * k + c:R_CAW + 4 * k + c + 1] for k in range(4)]
            cb = cols[:, R_CAB + c:R_CAB + c + 1]
            ts('dve', V[:, 0:NV], U[:, 0:NV], wc[0], cb, ALU.mult, ALU.add, [uk, 'cols'], [vk])
            for k in range(1, 4):
                stt('dve', V[:, 0:NV], U[:, k:k + NV], wc[k], V[:, 0:NV], ALU.mult, ALU.add,
                    [uk, vk, 'cols'], [vk])
            cp('dve', Vb[:, 0:NV], V[:, 0:NV], [vk], [vbk])

        def rnn_stageB(c):
            par = c % 2
            V, GA, Vb = Vp[par], GAp[par], Vbp[par]
            XB, XF = UXp[par], W[5]
            uk, vk, gk, vbk = UXk[par], Vk[par], GAk[par], 'Vb%d' % par
            for dd in range(2):
                X = XF if dd == 0 else XB
                XK = 'W5' if dd == 0 else uk
                TRb, TIb = (W[3], W[4]) if dd == 0 else (W[6], W[7])
                TRK, TIK = ('W3', 'W4') if dd == 0 else ('W6', 'W7')
                dc = dd * 4 + c
                for g, (dst, dk) in enumerate(((TRb, TRK), (TIb, TIK))):
                    slot = (dd * 2 + g) * 4 + c
                    hb = halfb[:, g * 8 + dc:g * 8 + dc + 1]
                    for (lo, hi) in blocks:
                        b = bank()
                        mm(psum[:, b, 0:hi - lo], Wbdb[:, slot, :], Vb[:, lo:hi], True, True,
                           ['Wbdb', vbk], ['ps%d' % b])
                        act(dst[:, lo:hi], psum[:, b, 0:hi - lo], AF.Tanh, ['ps%d' % b, 'halfb'], [dk],
                            scale=0.5, bias=hb)
                m4 = spc[:, dc, 0:1]
                m8 = spc[:, dc, 1:2]
                act(X[:, 0:NV], TRb[:, 0:NV], AF.Exp, [TRK, 'spc'], [XK], scale=m8, bias=m8)
                act(TRb[:, 0:NV], TRb[:, 0:NV], AF.Exp, [TRK, 'spc'], [TRK], scale=m4, bias=m4)
                act(X[:, 0:NV], X[:, 0:NV], AF.Sqrt, [XK, 'cst'], [XK], scale=-1.0, bias=one_c)
                stt('dve', TIb[:, 0:NV], TIb[:, 0:NV], 1.0, V[:, 0:NV], ALU.add, ALU.mult, [TIK, vk], [TIK])
                stt('dve', TIb[:, 0:NV], TIb[:, 0:NV], 0.5, X[:, 0:NV], ALU.mult, ALU.mult, [TIK, XK], [TIK])
                if dd == 0:
                    fw.op('dve', lambda e, X=X, TRb=TRb, TIb=TIb: e.tensor_tensor_scan(
                        out=X[:, 0:TC], data0=TRb[:, 0:TC], data1=TIb[:, 0:TC], initial=0.0,
                        op0=ALU.mult, op1=ALU.add), [TRK, TIK, XK], [XK])
                    fw.op('dve', lambda e, X=X, TRb=TRb, TIb=TIb: e.tensor_tensor_scan(
                        out=X[:, 259:NV], data0=TRb[:, 259:NV], data1=TIb[:, 259:NV], initial=X[:, TC - 1:TC],
                        op0=ALU.mult, op1=ALU.add), [TRK, TIK, XK], [XK])
                else:
                    fw.op('dve', lambda e, X=X, TRb=TRb, TIb=TIb: e.tensor_tensor_scan(
                        out=X[:, 0:TC][:, ::-1], data0=TRb[:, 0:TC][:, ::-1], data1=TIb[:, 0:TC][:, ::-1],
                        initial=0.0, op0=ALU.mult, op1=ALU.add), [TRK, TIK, XK], [XK])
                    fw.op('dve', lambda e, X=X, TRb=TRb, TIb=TIb: e.tensor_tensor_scan(
                        out=X[:, 259:NV][:, ::-1], data0=TRb[:, 259:NV][:, ::-1], data1=TIb[:, 259:NV][:, ::-1],
                        initial=X[:, 0:1], op0=ALU.mult, op1=ALU.add), [TRK, TIK, XK], [XK])
            L0, L1 = 259, NV
            tt('dve', XF[:, L0:L1], XF[:, L0:L1], XB[:, L0:L1], ALU.add, ['W5', uk], ['W5'])
            tt('dve', XF[:, L0:L1], XF[:, L0:L1], GA[:, 0:T], ALU.mult, ['W5', gk], ['W5'])
            SQ = W[3]
            act(SQ[:, 0:T], XF[:, L0:L1], AF.Square, ['W5'], ['W3'])
            ts('dve', yast, XF[:, L0:L1], cols[:, R_ONA + c:R_ONA + c + 1], None, ALU.mult, None,
               ['W5', 'cols'], ['hcb0'])
            dma('sp', ya_s[c], yast, ['hcb0'], [('ya_s', c)])

        def rnn_stageC(c):
            SQ = W[3]
            b = bank()
            for i in range(NT):
                mm(psum[:, b, i:i + 1], SQ[:, 128 * i:128 * i + 128], ones_f[:, 0:1], True, True,
                   ['W3', 'ones_f'], ['ps%d' % b])
            cp('dve', ssa[:, c, :], psum[:, b, 0:NT], ['ps%d' % b], ['ssa'])

        cp('dve', Wbdb, Wbd, ['Wbd'] + [('Wbdd', sl_) for sl_ in range(16)], ['Wbdb'])
        rnn_stageA(0)
        rnn_stageA(1)
        for c in range(4):
            rnn_stageB(c)
            if c + 2 < 4:
                rnn_stageA(c + 2)
            rnn_stageC(c)
        pos = st['pos']

        chk('rnn')
        for q in range(2):
            mset('dve', W[2 * q][:, 0:1], 0.0, ['W%d' % (2 * q)])
            mset('dve', W[2 * q][:, 2049:2050], 0.0, ['W%d' % (2 * q)])

        def hy_stageA(cc):
            q = cc % 2
            HY = W[2 * q]
            hk_ = 'W%d' % (2 * q)
            load_win(st['pos'] + 2)

            def ev_h(n, b):
                eng = 'dve' if n % 2 == 0 else 'act'
                cp(eng, HY[:, 1 + 512 * n:1 + 512 * n + 512], psum[:, b, :], ['ps%d' % b], [hk_])
            proj(cc, ev_h)
            st['pos'] += 1

        def hy_stageB(cc):
            g = (cc - 8) // 4
            c4 = (cc - 8) % 4
            q = cc % 2
            HY = W[2 * q]
            TMP = W[2 * q + 1]
            hk_ = 'W%d' % (2 * q)
            tk_ = 'W%d' % (2 * q + 1)
            wc = [cols[:, R_CBW + 12 * k + (cc - 8):R_CBW + 12 * k + (cc - 8) + 1] for k in range(3)]
            s = cc % 2
            ts('dve', TMP[:, 0:T], HY[:, 0:T], wc[0], None, ALU.mult, None, [hk_, 'cols'], [tk_])
            stt('dve', TMP[:, 0:T], HY[:, 1:1 + T], wc[1], TMP[:, 0:T], ALU.mult, ALU.add, [hk_, tk_, 'cols'], [tk_])
            stt('dve', hcb[s], HY[:, 2:2 + T], wc[2], TMP[:, 0:T], ALU.mult, ALU.add, [hk_, tk_, 'cols'],
                ['hcb%d' % s])
            for h in range(2):
                b = bank()
                psb = psum[:, b, :].bitcast(BF16)
                for i8 in range(8):
                    i = 8 * h + i8
                    tr(psb[:, 128 * i8:128 * i8 + 128], hcb[s][:, 128 * i:128 * i + 128], ident_b,
                       ['hcb%d' % s, 'ident_b'], ['ps%d' % b])
                cp('act' if h == 0 else 'dve', hyst[:, 8 * h:8 * h + 8, 128 * c4:128 * c4 + 128],
                   psb.rearrange("p (a b) -> p a b", a=8), ['ps%d' % b], ['hyst', 'W6', 'W7'])
            if c4 == 3:
                dma('sp', hy_s[g], hyst, ['hyst'], [('hy_s', g)])

        st['pos'] = pos
        hy_stageA(8)
        for cc in range(8, 20):
            if cc + 1 < 20:
                hy_stageA(cc + 1)
            hy_stageB(cc)
        fw.barrier()


        chk('A')
        Bq = Alloc(arena, P.end, NW)
        NS = 3
        cbuf = [Bq.bf16([128, 16, 128]) for _ in range(NS)]
        sbuf = [Bq.bf16([128, 16, 128]) for _ in range(NS)]
        NP1 = T + 1
        hdn2 = Bq.f32([65, 2052])
        w3aug = Bq.f32([65, 2048])
        w1t = Bq.f32([65, 64])
        w2t = Bq.f32([64, 64])
        fcols = Bq.f32([64, 4])
        delta = Bq.f32([128, 512])
        tsc = Bq.f32([128, 16, 2])
        psic = Bq.f32([128, 16, 3])
        smalli = Bq.i32([128, 16])
        fcol = Bq.f32([65, 2])
        fb_rep = Bq.f32([128, 2, 512])
        rnorm = Bq.f32([128, 512])
        dec = [Bq.f32([128, 512]) for _ in range(4)]
        kfb = [Bq.f32([128, 512]) for _ in range(4)]
        absum = [Bq.f32([128, 512]) for _ in range(2)]
        et = [Bq.f32([128, 512]) for _ in range(4)]
        xg = [Bq.bf16([128, 512]) for _ in range(2)]
        ymark = Bq.off
        SUM = Bq.bf16([128, 16, 512])
        DIF = Bq.bf16([128, 16, 512])
        Ut = Bq.bf16([128, 16, 512])
        Z1t = Bq.bf16([128, 16, 512])
        ybst = [SUM.rearrange("p a b -> p (a b)")[:, 0:T]]
        YA = Bq.bf16([128, 16, 512])
        YB = Bq.bf16([128, 16, 512])
        ssb = misc[:, 0:16]
        rsb = misc[:, 16:32]
        M = Alloc(arena, ymark, NW)
        zT = M.f32([65, 2052])
        h1 = M.f32([64, 2052])
        argb = M.f32([65, 2052])
        mtA = M.f32([65, 2052])
        mtB = M.f32([65, 2052])
        mtI = M.i32([65, 2052])
        posi = mtI

        dma('sp', fb_rep.rearrange("p a b -> p (a b)"), filt_bias_d.partition_broadcast(128), [], ['fb_rep'])
        mset('dve', w1t, 0.0, ['w1t'])
        dma('sp', w1t[0:16, :], filt_w1_d[1:17, :], ['w1t'], ['w1t_a'])
        dma('sp', w1t[32:48, :], filt_w1_d[17:33, :], ['w1t'], ['w1t_b'])
        dma('sp', w1t[64:65, :], filt_w1_d[0:1, :], ['w1t'], ['w1t_c'])
        dma('sp', w2t, filt_w2_d[:, :], [], ['w2t'])
        dma('sp', fcols, filt_cols_d[:, :], [], ['fcols'])
        dma('sp', w3aug[0:64, :], filt_w3_d[:, :], [], ['w3a'])
        dma('sp', w3aug[64:65, :], filt_b3_d[:, :], [], ['w3b'])

        n_ = float(T)
        iota(posi[:, 0:NP1], [[1, NP1]], 0, 0, ['mtI'])
        cp('dve', mtB[:, 0:NP1], posi[:, 0:NP1], ['mtI'], ['mtB'])
        iota(smalli[0:65, 0:1], [[0, 1]], 0, 1, ['smalli'])
        fw.op('dve', lambda e: e.tensor_single_scalar(out=smalli[0:65, 1:2], in_=smalli[0:65, 0:1], scalar=31,
                                                      op=ALU.bitwise_and), ['smalli'], ['smalli'])
        cp('dve', fcol[:, 0:1], smalli[0:65, 1:2], ['smalli'], ['fcol'])
        bstep = (15.0 - 1e-4) / 15.0
        ts('dve', fcol[:, 1:2], fcol[:, 0:1], bstep * TWO_PI / n_, 1e-4 * TWO_PI / n_, ALU.mult, ALU.add,
           ['fcol'], ['fcol'])
        ts('dve', argb[0:64, 0:NP1], mtB[0:64, 0:NP1], fcol[0:64, 1:2], None, ALU.mult, None,
           ['mtB', 'fcol'], ['argb'])
        mset('dve', zT[:, 0:NP1], 0.0, ['zT'])
        sin_reduce(zT[0:32, 0:NP1], argb[0:32, 0:NP1], 32, None, 0.25, mtA[0:32, 0:NP1], h1[0:32, 0:NP1],
                   mtI[0:32, 0:NP1], ['argb', 'zT'], ['zT'], 'mz0')
        sin_reduce(zT[32:64, 0:NP1], argb[32:64, 0:NP1], 32, 32, 0.5, mtA[32:64, 0:NP1], h1[32:64, 0:NP1],
                   mtI[32:64, 0:NP1], ['argb', 'zT'], ['zT'], 'mz1')
        ts('dve', zT[64:65, 0:NP1], mtB[64:65, 0:NP1], 1.0 / (n_ - 1.0), None, ALU.mult, None, ['mtB', 'zT'], ['zT'])
        pblocks = [(0, 512), (512, 1024), (1024, 1536), (1536, 2048), (2048, NP1)]
        for (lo, hi) in pblocks:
            b = bank()
            mm(psum[0:64, b, 0:hi - lo], w1t, zT[:, lo:hi], True, True,
               ['w1t', 'w1t_a', 'w1t_b', 'w1t_c', 'zT'], ['ps%d' % b])
            ts('dve', argb[0:64, lo:hi], psum[0:64, b, 0:hi - lo], fcols[:, 0:1], fcols[:, 1:2], ALU.add, ALU.mult,
               ['ps%d' % b, 'fcols'], ['argb'])
        sin_reduce(h1[:, 0:NP1], argb[0:64, 0:NP1], 64, None, 0.0, mtA[0:64, 0:NP1], zT[0:64, 0:NP1],
                   mtI[0:64, 0:NP1], ['argb', 'zT'], ['h1'], 'mz2')
        for (lo, hi) in pblocks:
            b = bank()
            mm(psum[0:64, b, 0:hi - lo], w2t, h1[:, lo:hi], True, True, ['w2t', 'h1'], ['ps%d' % b])
            ts('dve', argb[0:64, lo:hi], psum[0:64, b, 0:hi - lo], fcols[:, 2:3], fcols[:, 3:4], ALU.add, ALU.mult,
               ['ps%d' % b, 'fcols'], ['argb'])
        sin_reduce(hdn2[0:64, 0:NP1], argb[0:64, 0:NP1], 64, None, 0.0, mtA[0:64, 0:NP1], zT[0:64, 0:NP1],
                   mtI[0:64, 0:NP1], ['argb', 'h1'], ['hdn2'], 'mz3')
        mset('dve', hdn2[64:65, 0:NP1], 1.0, ['hdn2'])
        mset('dve', hdn2[:, T:NP1], 0.0, ['hdn2'])
        dma('sp', Ut, hy_s[0], [('hy_s', 0), 'hdn2'], ['Ut'])

        chk('mlp')
        dstep_lo = abs(math.log(1e-2) / 1.5)
        dstep_hi = abs(math.log(1e-2) / 0.3)
        di = et[0].bitcast(I32)
        iota(di, [[1, 512]], 0, 0, ['et0'])
        cp('dve', delta, di, ['et0'], ['delta'])
        ts('dve', delta, delta, (dstep_hi - dstep_lo) / 511.0, dstep_lo, ALU.mult, ALU.add, ['delta'], ['delta'])
        iota(smalli, [[128, 16]], 0, 1, ['smalli'])
        cp('dve', tsc[:, :, 0], smalli, ['smalli'], ['tsc'])
        ts('dve', tsc[:, :, 1], tsc[:, :, 0], 1.0, -1.0 / (n_ - 1.0), ALU.add, ALU.mult, ['tsc'], ['tsc'])
        ts('dve', tsc[:, :, 0], tsc[:, :, 0], -1.0 / (n_ - 1.0), None, ALU.mult, None, ['tsc'], ['tsc'])
        iota(smalli, [[256, 16]], 1, 2, ['smalli'])
        act(psic[:, :, 1], smalli, AF.Sin, ['smalli', 'cst'], ['psic'], scale=TWO_PI / 16384.0, bias=negpi)
        fw.op('dve', lambda e: e.tensor_single_scalar(out=smalli, in_=smalli, scalar=4096, op=ALU.add),
              ['smalli', 'psic'], ['smalli'])
        act(psic[:, :, 0], smalli, AF.Sin, ['smalli', 'cst'], ['psic'], scale=TWO_PI / 16384.0, bias=negpi)
        ts('dve', psic[:, :, 2], psic[:, :, 0], -1.0, None, ALU.mult, None, ['psic'], ['psic'])
        mset('dve', ssb, 0.0, ['ssb'])

        st['dpos'] = 0

        def load_dft(j):
            s = st['dpos'] % NS
            st['dpos'] += 1
            dma('sp', cbuf[s], dftc_s[j], [('dftc', j)], ['cbuf%d' % s])
            dma('sp', sbuf[s], dfts_s[j], [('dfts', j)], ['sbuf%d' % s])
            return s

        for o in range(2):
            Uin = Ut if o == 0 else Z1t
            UK = 'Ut' if o == 0 else 'Z1t'
            st['nb'] = 7
            st['psrr'] = 0
            for i in range(NT):
                b1 = bank()
                mm(psum[:, b1, :], hdn2[:, 128 * i:128 * i + 128], w3aug[:, 512 * o:512 * o + 512], True, True,
                   ['hdn2', 'w3a', 'w3b'], ['ps%d' % b1])
                b2 = bank()
                mm(psum[:, b2, :], hdn2[:, 128 * i + 1:128 * i + 129], w3aug[:, 1024 + 512 * o:1024 + 512 * o + 512],
                   True, True, ['hdn2', 'w3a', 'w3b'], ['ps%d' % b2])
                q = i % 2
                d0, d1, k0, k1, ab = dec[2 * q], dec[2 * q + 1], kfb[2 * q], kfb[2 * q + 1], absum[q]
                kd0, kd1, kk0, kk1, kab = 'dec%d' % (2 * q), 'dec%d' % (2 * q + 1), 'kf%d' % q, 'kb%d' % q, 'absum%d' % q
                act(d0, delta, AF.Exp, ['delta', 'tsc'], [kd0], scale=tsc[:, i, 0:1])
                act(d1, delta, AF.Exp, ['delta', 'tsc'], [kd1], scale=tsc[:, i, 1:2])
                tt('dve', k0, psum[:, b1, :], d0, ALU.mult, ['ps%d' % b1, kd0], [kk0])
                tt('dve', k1, psum[:, b2, :], d1, ALU.mult, ['ps%d' % b2, kd1], [kk1])
                tt('dve', SUM[:, i, :], k0, k1, ALU.add, [kk0, kk1], [('SUM', i)])
                tt('dve', DIF[:, i, :], k0, k1, ALU.subtract, [kk0, kk1], [('DIF', i)])
                stt('dve', k0, k0, -1.0, k0, ALU.mult, ALU.max, [kk0], [kk0])
                stt('dve', k1, k1, -1.0, k1, ALU.mult, ALU.max, [kk1], [kk1])
                tt('dve', ab, k0, k1, ALU.add, [kk0, kk1], [kab])
                if i > 0:
                    qp = (i - 1) % 2
                    mm(psum[:, 7, :], ones_f, absum[qp], i == 1, False, ['ones_f', 'absum%d' % qp], ['ps7'])
            mm(psum[:, 7, :], ones_f, absum[(NT - 1) % 2], False, True, ['ones_f', 'absum%d' % ((NT - 1) % 2)], ['ps7'])
            fw.op('dve', lambda e: e.reciprocal(out=rnorm, in_=psum[:, 7, :]), ['ps7'], ['rnorm'])
            st['nb'] = 8
            sl = {}
            sl[0] = load_dft(0)
            sl[1] = load_dft(1)
            SK = [('SUM', i) for i in range(NT)]
            DK = [('DIF', i) for i in range(NT)]
            for j in range(NT):
                if j + 2 < NT:
                    sl[j + 2] = load_dft(j + 2)
                s = sl[j]
                bGA, bGB, bA, bB = bank(), bank(), bank(), bank()
                for i in range(NT):
                    mm(psum[:, bGA, :], cbuf[s][:, i, :], SUM[:, i, :], i == 0, i == NT - 1,
                       ['cbuf%d' % s] + SK, ['ps%d' % bGA])
                    mm(psum[:, bA, :], cbuf[s][:, i, :], Uin[:, i, :], i == 0, i == NT - 1,
                       ['cbuf%d' % s, UK], ['ps%d' % bA])
                for i in range(NT):
                    mm(psum[:, bGB, :], sbuf[s][:, i, :], DIF[:, i, :], i == 0, i == NT - 1,
                       ['sbuf%d' % s] + DK, ['ps%d' % bGB])
                    mm(psum[:, bB, :], sbuf[s][:, i, :], Uin[:, i, :], i == 0, i == NT - 1,
                       ['sbuf%d' % s, UK], ['ps%d' % bB])
                ncos = psic[:, j, 0:1]
                nsin = psic[:, j, 1:2]
                pcos = psic[:, j, 2:3]
                kGA, kGB, kA, kB = 'ps%d' % bGA, 'ps%d' % bGB, 'ps%d' % bA, 'ps%d' % bB
                act(et[0], psum[:, bGA, :], AF.Copy, [kGA, 'psic'], ['et0'], scale=ncos)
                stt('dve', et[0], psum[:, bGB, :], nsin, et[0], ALU.mult, ALU.add, [kGB, 'psic', 'et0'], ['et0'])
                tt('dve', et[0], et[0], rnorm, ALU.mult, ['et0', 'rnorm'], ['et0'])
                act(et[1], psum[:, bGA, :], AF.Copy, [kGA, 'psic'], ['et1'], scale=nsin)
                stt('dve', et[1], psum[:, bGB, :], pcos, et[1], ALU.mult, ALU.add, [kGB, 'psic', 'et1'], ['et1'])
                tt('dve', et[1], et[1], rnorm, ALU.mult, ['et1', 'rnorm'], ['et1'])
                tt('dve', et[2], psum[:, bA, :], et[0], ALU.mult, [kA, 'et0'], ['et2'])
                tt('dve', et[3], psum[:, bB, :], et[1], ALU.mult, [kB, 'et1'], ['et3'])
                tt('dve', YA[:, j, :], et[2], et[3], ALU.add, ['et2', 'et3'], [('YA', j)])
                tt('dve', et[2], psum[:, bB, :], et[0], ALU.mult, [kB, 'et0', 'et2'], ['et2'])
                tt('dve', et[3], psum[:, bA, :], et[1], ALU.mult, [kA, 'et1', 'et3'], ['et3'])
                tt('dve', YB[:, j, :], et[2], et[3], ALU.subtract, ['et2', 'et3'], [('YB', j)])
            YAK = [('YA', i) for i in range(NT)]
            YBK = [('YB', i) for i in range(NT)]
            sl = {}
            sl[0] = load_dft(0)
            sl[1] = load_dft(1)
            for j in range(NT):
                if j + 2 < NT:
                    sl[j + 2] = load_dft(j + 2)
                s = sl[j]
                g2 = j % 2
                dma('sp', xg[g2], hy_s[1 + o][:, j, :], [('hy_s', 1 + o)], ['xg%d' % g2])
                b = bank()
                for i in range(NT):
                    mm(psum[:, b, :], cbuf[s][:, i, :], YA[:, i, :], i == 0, False, ['cbuf%d' % s] + YAK, ['ps%d' % b])
                for i in range(NT):
                    mm(psum[:, b, :], sbuf[s][:, i, :], YB[:, i, :], False, i == NT - 1, ['sbuf%d' % s] + YBK,
                       ['ps%d' % b])
                tt('dve', et[0], Uin[:, j, :], fb_rep[:, o, :], ALU.mult, [UK, 'fb_rep'], ['et0'])
                stt('dve', et[0], psum[:, b, :], 2.0 / 4096.0, et[0], ALU.mult, ALU.add, ['ps%d' % b, 'et0'], ['et0'])
                if o == 0:
                    tt('dve', Z1t[:, j, :], et[0], xg[g2], ALU.mult, ['et0', 'xg%d' % g2], ['Z1t'])
                else:
                    tt('dve', et[1], et[0], xg[g2], ALU.mult, ['et0', 'xg%d' % g2], ['et1'])
                    act(et[2], et[1], AF.Square, ['et1'], ['et2', 'ssb'], accum=ssb[:, j:j + 1])
                    cp('dve', Ut[:, j, :], et[1], ['et1'], ['Ut'])
        chk('conv')
        act(rsb, ssb, AF.Sqrt, ['ssb', 'cst'], ['rsb'], scale=1.0 / 512.0, bias=eps_c)
        fw.op('dve', lambda e: e.reciprocal(out=rstd_b, in_=rsb), ['rsb'], ['rstd_b'])
        for c in range(4):
            for h in range(2):
                b = bank()
                psb = psum[:, b, :].bitcast(BF16)
                for i8 in range(8):
                    i = 8 * h + i8
                    tr(psb[:, 128 * i8:128 * i8 + 128], Ut[:, i, 128 * c:128 * c + 128], ident_b,
                       ['Ut', 'ident_b'], ['ps%d' % b])
                ts('dve' if h == 0 else 'dve', ybst[0][:, 1024 * h:1024 * h + 1024], psb,
                   cols[:, R_ONB + c:R_ONB + c + 1], None, ALU.mult, None, ['ps%d' % b, 'cols'], ['ybst'])
            dma('sp', yb_s[c], ybst[0], ['ybst'], [('yb_s', c)])
        if debug:
            dma('sp', dbg_s[2][:, 0:512], rnorm, ['rnorm'], ['dbg2'])
        fw.barrier()


        chk('B')
        Cq = Alloc(arena, P.end, NW)
        X1 = Cq.f32([128, NT, D])
        h2T = Cq.bf16([128, 8, T])
        Lg = Cq.f32([128, NT, 36])
        comb = Cq.f32([128, NT, 32])
        fing = Cq.f32([128, D])
        wr = Cq.f32([128, 8, 36])
        br = Cq.f32([1, 36])
        ss2 = Cq.f32([128, 16])
        rs2 = Cq.f32([128, 16])
        rstd2 = Cq.f32([128, 16])
        wmark = Cq.off
        wg = [Cq.bf16([128, 8, DE]) for _ in range(2)]
        wu = [Cq.bf16([128, 8, DE]) for _ in range(2)]
        wd = [Cq.bf16([128, 4, D]) for _ in range(2)]
        tmark = Cq.off
        acth = [Cq.bf16([128, 4, 512]) for _ in range(2)]
        sg = [Cq.f32([128, 512]) for _ in range(2)]
        junkf = Cq.bf16([128, D])
        Mq = Alloc(arena, wmark, NW)
        yab = Mq.bf16([128, 8, T])
        wo = Mq.bf16([128, 8, D])
        xt2 = [Mq.f32([128, D]) for _ in range(3)]
        yts = [Mq.f32([128, D]) for _ in range(2)]
        h2f = [Mq.f32([128, 8, 128]) for _ in range(3)]
        junk2 = Mq.bf16([128, D])

        for c in range(4):
            dma('sp', yab[:, c, :], ya_s[c], [('ya_s', c)], [('yab', c)])
            dma('sp', yab[:, 4 + c, :], yb_s[c], [('yb_s', c)], [('yab', 4 + c)])
        dma('pool', wo, w_out_d.rearrange("(k p) n -> p k n", p=128), [], ['wo'])
        for k in range(8):
            dma('sp', wr[:, k, :], w_r_d[128 * k:128 * k + 128, :], [], ['wr'])
        dma('sp', br, b_r_d[:, :], [], ['br'])
        dma('sp', fing, final_g_d.partition_broadcast(128), [], ['fing'])
        chk('cdma')
        tt('dve', ssa[:, 0, :], ssa[:, 0, :], ssa[:, 1, :], ALU.add, ['ssa'], ['ssa'])
        tt('dve', ssa[:, 2, :], ssa[:, 2, :], ssa[:, 3, :], ALU.add, ['ssa'], ['ssa'])
        tt('dve', ssa[:, 0, :], ssa[:, 0, :], ssa[:, 2, :], ALU.add, ['ssa'], ['ssa'])
        act(ssa[:, 1, :], ssa[:, 0, :], AF.Sqrt, ['ssa', 'cst'], ['ssa'], scale=1.0 / 512.0, bias=eps_c)
        fw.op('dve', lambda e: e.reciprocal(out=rstd_a, in_=ssa[:, 1, :]), ['ssa'], ['rstd_a'])
        mset('dve', ss2, 0.0, ['ss2'])
        chk('crstd')
        st['nb'] = 4
        st['psrr'] = 0
        st['pair'] = 0

        def pair2():
            p = st.get('pair2', 0)
            st['pair2'] = 1 - p
            return 4 + 2 * p

        yabk = [('yab', c) for c in range(8)]
        g1v = g_rep[:, 0:2, :]
        g2v = g_rep[:, 2:4, :]
        def mg_stage1(i):
            s = i % 3
            xk = 'xt2%d' % s
            dma('sp', xt2[s], xpe_s[i], [('xpe', i)], [xk])
            pa = pair2()
            pbk = pair2()
            for half in range(2):
                for c in range(4):
                    mm(psum[:, pa + half, :], yab[:, c, 128 * i:128 * i + 128], wo[:, c, 512 * half:512 * half + 512],
                       c == 0, c == 3, yabk + ['wo'], ['ps%d' % (pa + half)])
                for c in range(4):
                    mm(psum[:, pbk + half, :], yab[:, 4 + c, 128 * i:128 * i + 128],
                       wo[:, 4 + c, 512 * half:512 * half + 512], c == 0, c == 3, yabk + ['wo'], ['ps%d' % (pbk + half)])
            yt_ = yts[i % 2]
            yk = 'yt%d' % (i % 2)
            ytv = yt_.rearrange("p (a b) -> p a b", a=2)
            act(ytv, psum[:, pa:pa + 2, :], AF.Copy, ['ps%d' % pa, 'ps%d' % (pa + 1), 'rstd_a'], [yk],
                scale=rstd_a[:, i:i + 1])
            stt('dve', ytv, psum[:, pbk:pbk + 2, :], rstd_b[:, i:i + 1], ytv, ALU.mult, ALU.add,
                ['ps%d' % pbk, 'ps%d' % (pbk + 1), 'rstd_b', yk], [yk])
            tt('dve', ytv, ytv, g1v, ALU.mult, [yk, 'g_rep'], [yk])
            X1i = X1[:, i, :]
            tt('dve', X1i, xt2[s], yt_, ALU.add, [xk, yk], [('X1', i)])
            act(junk2, X1i, AF.Square, [('X1', i)], ['junk2', 'ss2'], accum=ss2[:, i:i + 1])
            act(rs2[:, i:i + 1], ss2[:, i:i + 1], AF.Sqrt, ['ss2', 'cst'], ['rs2'], scale=1.0 / D, bias=eps_c)
            fw.op('dve', lambda e, i=i: e.reciprocal(out=rstd2[:, i:i + 1], in_=rs2[:, i:i + 1]), ['rs2'], ['rstd2'])
            ts('dve', xt2[s], X1i, rstd2[:, i:i + 1], None, ALU.mult, None, [('X1', i), 'rstd2', xk], [xk])

        def mg_stage2(i):
            s = i % 3
            xk = 'xt2%d' % s
            pb = 2 * (i % 2)
            pk = ['ps%d' % pb, 'ps%d' % (pb + 1)]
            psT = psum[:, pb:pb + 2, :].rearrange("p a (b c) -> p (a b) c", c=128)
            for k in range(8):
                tr(psT[:, k, :], xt2[s][:, 128 * k:128 * k + 128], ident_f, [xk, 'ident_f'], pk)
            hk = 'h2f%d' % s
            for k in range(8):
                if k < 4:
                    act(h2f[s][:, k, :], psT[:, k, :], AF.Identity, [pk[0], 'scl2', 'modc'], [hk],
                        scale=scl2[:, k:k + 1], bias=modc[:, 16 + k, 0:1])
                else:
                    ts('dve', h2f[s][:, k, :], psT[:, k, :], scl2[:, k:k + 1], modc[:, 16 + k, 0:1], ALU.mult, ALU.add,
                       [pk[1], 'scl2', 'modc'], [hk])
            cp('dve', h2T[:, :, 128 * i:128 * i + 128], h2f[s], [hk], [('h2T', i)])

        def mg_stage2b(i):
            s = i % 3
            hk = 'h2f%d' % s
            b = 2 * (i % 2)
            for k in range(8):
                mm(psum[:, b, 0:36], h2f[s][:, k, :], wr[:, k, :], k == 0, False, [hk, 'wr'], ['ps%d' % b])
            mm(psum[:, b, 0:36], ones_f[0:1, :], br[0:1, :], False, True, ['ones_f', 'br'], ['ps%d' % b])
            cp('dve', Lg[:, i, :], psum[:, b, 0:36], ['ps%d' % b], ['Lg'])

        mg_stage1(0)
        mg_stage1(1)
        for i in range(NT):
            mg_stage2(i)
            if i + 2 < NT:
                mg_stage1(i + 2)
            mg_stage2b(i)
        if debug:
            dma('sp', dbg_s[3][:, 0:576], Lg.rearrange("p a b -> p (a b)"), ['Lg'], ['dbg3'])
        fw.barrier()

        chk('merge')
        wgv = w_gate_d.rearrange("e (k p) n -> e p k n", p=128)
        wuv = w_up_d.rearrange("e (k p) n -> e p k n", p=128)
        wdv = w_down_d.rearrange("e (k p) n -> e p k n", p=128)

        def load_expert(e_):
            if e_ >= NE:
                return
            s = e_ % 2
            dma('pool', wg[s], wgv[e_], [], ['wg%d' % s])
            dma('pool', wu[s], wuv[e_], [], ['wu%d' % s])
            dma('pool', wd[s], wdv[e_], [], ['wd%d' % s])

        load_expert(0)
        load_expert(1)
        Rq = Alloc(arena, Cq.off, NW)
        gmax = Rq.f32([128, NT])
        goh = Rq.f32([128, NT, 4])
        gex = Rq.f32([128, NT, 4])
        gsum = Rq.f32([128, NT])
        gp = Rq.f32([128, NT])
        esel = Rq.f32([128, NT, 8])
        etmp = Rq.f32([128, NT, 8])
        oh1 = Rq.f32([128, NT, 8])
        oh2 = Rq.f32([128, NT, 8])
        msk = Rq.f32([128, NT, 8])
        m1 = Rq.f32([128, NT])
        m2 = Rq.f32([128, NT])
        dlt = Rq.f32([128, NT])
        w1 = Rq.f32([128, NT])
        w2 = Rq.f32([128, NT])
        c8 = Rq.f32([128, NT, 8])
        gl = Lg[:, :, 0:4]
        el = Lg[:, :, 4:36].rearrange("p a (g e) -> p a g e", g=4)

        def bc(ap, n):
            return ap.unsqueeze(2).to_broadcast([128, NT, n])

        def dv(fn, r, w):
            fw.op('dve', fn, r, w)
        dv(lambda e: e.tensor_reduce(out=gmax, in_=gl, axis=AX.X, op=ALU.max), ['Lg'], ['gmax'])
        tt('dve', goh, gl, bc(gmax, 4), ALU.is_equal, ['Lg', 'gmax'], ['goh'])
        tt('dve', gex, gl, bc(gmax, 4), ALU.subtract, ['Lg', 'gmax'], ['gex'])
        act(gex, gex, AF.Exp, ['gex'], ['gex'])
        dv(lambda e: e.tensor_reduce(out=gsum, in_=gex, axis=AX.X, op=ALU.add), ['gex'], ['gsum'])
        dv(lambda e: e.reciprocal(out=gp, in_=gsum), ['gsum'], ['gp'])
        tt('dve', esel, el[:, :, 0, :], bc(goh[:, :, 0], 8), ALU.mult, ['Lg', 'goh'], ['esel'])
        for g in range(1, 4):
            tt('dve', etmp, el[:, :, g, :], bc(goh[:, :, g], 8), ALU.mult, ['Lg', 'goh', 'esel'], ['etmp'])
            tt('dve', esel, esel, etmp, ALU.add, ['esel', 'etmp'], ['esel'])
        dv(lambda e: e.tensor_reduce(out=m1, in_=esel, axis=AX.X, op=ALU.max), ['esel'], ['m1'])
        tt('dve', oh1, esel, bc(m1, 8), ALU.is_equal, ['esel', 'm1'], ['oh1'])
        stt('dve', msk, oh1, -1e30, esel, ALU.mult, ALU.add, ['oh1', 'esel'], ['msk'])
        dv(lambda e: e.tensor_reduce(out=m2, in_=msk, axis=AX.X, op=ALU.max), ['msk'], ['m2'])
        tt('dve', oh2, msk, bc(m2, 8), ALU.is_equal, ['msk', 'm2'], ['oh2'])
        tt('dve', dlt, m2, m1, ALU.subtract, ['m1', 'm2'], ['dlt'])
        act(dlt, dlt, AF.Exp, ['dlt'], ['dlt'])
        ts('dve', w1, dlt, 1.0, None, ALU.add, None, ['dlt'], ['w1'])
        dv(lambda e: e.reciprocal(out=w1, in_=w1), ['w1'], ['w1'])
        tt('dve', w1, w1, gp, ALU.mult, ['w1', 'gp'], ['w1'])
        tt('dve', w2, w1, dlt, ALU.mult, ['w1', 'dlt'], ['w2'])
        tt('dve', c8, oh1, bc(w1, 8), ALU.mult, ['oh1', 'w1'], ['c8'])
        tt('dve', etmp, oh2, bc(w2, 8), ALU.mult, ['oh2', 'w2', 'esel'], ['etmp'])
        tt('dve', c8, c8, etmp, ALU.add, ['c8', 'etmp'], ['c8'])
        comb4 = comb.rearrange("p a (g e) -> p a g e", g=4)
        for g in range(4):
            tt('dve', comb4[:, :, g, :], c8, bc(goh[:, :, g], 8), ALU.mult, ['c8', 'goh'], ['comb'])
        if debug:
            dma('sp', dbg_s[4][:, 0:512], comb.rearrange("p a b -> p (a b)"), ['comb'], ['dbg4'])

        chk('route')
        outk = []

        def final_tile(i):
            X1i = X1[:, i, :]
            act(junkf, X1i, AF.Square, [('X1', i)], ['junkf', 'ss2'], accum=ss2[:, i:i + 1])
            act(rs2[:, i:i + 1], ss2[:, i:i + 1], AF.Sqrt, ['ss2', 'cst'], ['rs2'], scale=1.0 / D, bias=eps_c)
            fw.op('dve', lambda e, i=i: e.reciprocal(out=rstd2[:, i:i + 1], in_=rs2[:, i:i + 1]), ['rs2'], ['rstd2'])
            stt('dve', X1i, X1i, rstd2[:, i:i + 1], fing, ALU.mult, ALU.mult, [('X1', i), 'rstd2', 'fing'], [('X1', i)])
            dma('sp', out_d[128 * i:128 * i + 128, :], X1i, [('X1', i)], [('out', i)])
            outk.append(('out', i))

        mset('dve', ss2, 0.0, ['ss2'])
        h2k = [[('h2T', 4 * n + j) for j in range(4)] for n in range(4)]
        st['nb'] = 4
        st['psrr'] = 0
        ai = 0
        pending = []

        def flush():
            while pending:
                pending.pop(0)()

        for e_ in range(NE):
            s = e_ % 2
            for f in range(4):
                tt('dve', wd[s][:, f, :].rearrange("p (a b) -> p a b", a=2), wd[s][:, f, :].rearrange("p (a b) -> p a b", a=2),
                   g2v, ALU.mult, ['wd%d' % s, 'g_rep'], ['wd%d' % s])
            for n in range(4):
                a = ai % 2
                ai += 1
                ak = 'acth%d' % a
                for f in range(4):
                    bg = bank()
                    for k in range(8):
                        mm(psum[:, bg, :], wg[s][:, k, 128 * f:128 * f + 128], h2T[:, k, 512 * n:512 * n + 512],
                           k == 0, k == 7, ['wg%d' % s] + h2k[n], ['ps%d' % bg])
                    bu = bank()
                    for k in range(8):
                        mm(psum[:, bu, :], wu[s][:, k, 128 * f:128 * f + 128], h2T[:, k, 512 * n:512 * n + 512],
                           k == 0, k == 7, ['wu%d' % s] + h2k[n], ['ps%d' % bu])
                    r_ = f % 2
                    act(sg[r_], psum[:, bg, :], AF.Silu, ['ps%d' % bg], ['sg%d' % r_])
                    tt('dve', acth[a][:, f, :], sg[r_], psum[:, bu, :], ALU.mult, ['sg%d' % r_, 'ps%d' % bu], [ak])
                    if f == 0:
                        flush()

                def down(e_=e_, s=s, n=n, a=a, ak=ak):
                    for ti in range(4):
                        i = 4 * n + ti
                        pd = pair2()
                        for half in range(2):
                            for f in range(4):
                                mm(psum[:, pd + half, :], acth[a][:, f, 128 * ti:128 * ti + 128],
                                   wd[s][:, f, 512 * half:512 * half + 512], f == 0, f == 3,
                                   [ak, 'wd%d' % s], ['ps%d' % (pd + half)])
                        X1v = X1[:, i, :].rearrange("p (a b) -> p a b", a=2)
                        stt('dve', X1v, psum[:, pd:pd + 2, :], comb[:, i, e_:e_ + 1], X1v, ALU.mult, ALU.add,
                            ['ps%d' % pd, 'ps%d' % (pd + 1), 'comb', ('X1', i)], [('X1', i)])
                        if e_ == NE - 1:
                            final_tile(i)
                    if n == 3:
                        load_expert(e_ + 2)
                pending.append(down)
        flush()

        chk('moe')
        fw.op('sp', None, reads=outk)

        counts = fw.emit()
    return nc, counts


_PROG = {}


def _get_program(debug=False, stop=None):
    if (debug, stop) not in _PROG:
        _PROG[(debug, stop)] = build_program(debug, stop)
    return _PROG[(debug, stop)]


def _core_inputs(inp, b):
    f = lambda a: np.ascontiguousarray(np.asarray(a, dtype=np.float32))
    rows = np.concatenate([
        f(inp["c"])[b].reshape(8, 128),
        f(inp["c_ctx"]).reshape(8, 128),
        f(inp["b_ada"])[0].reshape(48, 128),
        f(inp["norm1_g"])[0].reshape(8, 128),
        f(inp["norm2_g"])[0].reshape(8, 128),
        f(inp["conv_a_w"])[0].reshape(16, 128),
        f(inp["conv_a_b"])[0].reshape(4, 128),
        f(inp["lru_ba"])[0].reshape(8, 128),
        f(inp["lru_bx"])[0].reshape(8, 128),
        f(inp["lru_lambda"])[0].reshape(8, 128),
        f(inp["conv_b_w"])[0].reshape(36, 128),
        f(inp["out_norm_a"])[0].reshape(4, 128),
        f(inp["out_norm_b"])[0].reshape(4, 128),
    ], axis=0)
    assert rows.shape == (NR, 128)
    return {
        "x": f(inp["x"])[b],
        "ctx": f(inp["ctx"])[b],
        "rows": f(rows),
        "w_ada": f(inp["w_ada"])[0],
        "b_ada": f(inp["b_ada"])[0].reshape(1, 6 * D),
        "w_in": f(inp["w_in"])[0],
        "lru_wa": f(inp["lru_wa"])[0].reshape(16, 64, 64),
        "lru_wx": f(inp["lru_wx"])[0].reshape(16, 64, 64),
        "filt_w1": f(inp["filt_w1"])[0],
        "filt_cols": f(np.stack([f(inp["filt_b1"])[0], f(inp["filt_freq1"])[0],
                                 f(inp["filt_b2"])[0], f(inp["filt_freq2"])[0]], axis=1)),
        "filt_w2": f(inp["filt_w2"])[0],
        "filt_w3": f(inp["filt_w3"])[0],
        "filt_b3": f(inp["filt_b3"])[0].reshape(1, 2048),
        "filt_bias": f(inp["filt_bias"])[0].reshape(1, 1024),
        "w_out": f(inp["w_out"])[0],
        "w_r": f(np.concatenate([f(inp["w_rg"])[0], f(inp["w_re"])[0]], axis=1)),
        "b_r": f(np.concatenate([f(inp["b_rg"])[0], f(inp["b_re"])[0]], axis=0)).reshape(1, 36),
        "w_gate": f(inp["w_gate"])[0],
        "w_up": f(inp["w_up"])[0],
        "w_down": f(inp["w_down"])[0],
        "final_g": f(inp["final_g"]).reshape(1, D),
    }


def kernel(**inputs):
    nc, _ = _get_program(False)
    nb = int(np.asarray(inputs["x"]).shape[0])
    shared = None
    in_maps = []
    for b in range(nb):
        m = _core_inputs(inputs, b)
        if shared is None:
            shared = m
        else:
            for k in m:
                if k not in ("x", "ctx", "rows"):
                    m[k] = shared[k]
        in_maps.append(m)
    res = run_bass_kernel_spmd(nc, in_maps, core_ids=list(range(nb)))
    return np.stack([np.asarray(r["out"], dtype=np.float32) for r in res.results], axis=0)
```

```python
import math
from contextlib import ExitStack
import numpy as np
import concourse.bass as bass
import concourse.mybir as mybir
from concourse.bass_utils import run_bass_kernel_spmd

F32 = mybir.dt.float32
BF16 = mybir.dt.bfloat16
I32 = mybir.dt.int32
AF = mybir.ActivationFunctionType
ALU = mybir.AluOpType
AX = mybir.AxisListType

D = 1024
T = 2048
TC = 256
NT = 16
EPS = 1e-6
NE = 32
DE = 512
TWO_PI = 2.0 * math.pi
VAR = ''

R_C, R_CC, R_BADA, R_N1, R_N2, R_CAW, R_CAB, R_BA, R_BX, R_LAM, R_CBW, R_ONA, R_ONB = (
    0, 8, 16, 64, 72, 80, 96, 100, 108, 116, 124, 160, 164)
NR = 168


class FW:
    ENG = ("pe", "act", "dve", "pool", "sp")
    SEG = 16000
    NDMA = 28
    NHW = 20

    def __init__(self, nc, es):
        self.nc = nc
        self.es = es
        self.ops = {e: [] for e in self.ENG}
        self.lastw = {}
        self.readers = {}
        self.waited = {e: {} for e in self.ENG}
        self.dma_cnt = [0] * self.NDMA
        self.dma_rr = 0
        self.dma_rr_sw = 0
        self.enabled = True

    def op(self, eng, fn, reads=(), writes=(), dma=False):
        if not self.enabled:
            return None
        pr = [k for k in reads if isinstance(k, str) and k[:2] == 'ps' and k[2:].isdigit()]
        idx = len(self.ops[eng])
        need = {}

        def add(tok):
            if tok[0] == 'c':
                if tok[1] == eng and eng == 'pe':
                    return
                key = ('c', tok[1])
            else:
                key = ('d', tok[1])
            if need.get(key, -1) < tok[2]:
                need[key] = tok[2]

        for k in reads:
            t = self.lastw.get(k)
            if t is not None:
                add(t)
        for k in pr:
            for r in self.readers.get(k, ()):
                if r[0] == 'c' and r[1] != eng:
                    add(r)
        for k in writes:
            t = self.lastw.get(k)
            if t is not None:
                add(t)
            for r in self.readers.get(k, ()):
                add(r)
        waits = []
        wd = self.waited[eng]
        for key, val in need.items():
            if wd.get(key, -1) >= val:
                continue
            wd[key] = val
            waits.append((key[0], key[1], val))
        if dma:
            if eng == 'pool':
                s = self.NHW + self.dma_rr_sw
                self.dma_rr_sw = (self.dma_rr_sw + 1) % (self.NDMA - self.NHW)
            else:
                s = self.dma_rr
                self.dma_rr = (s + 1) % self.NHW
            prev = self.dma_cnt[s]
            if prev > 0 and wd.get(('d', s), -1) < prev:
                wd[('d', s)] = prev
                waits.append(('d', s, prev))
            self.dma_cnt[s] = prev + 16
            tok = ('d', s, prev + 16)
        else:
            tok = ('c', eng, idx)
        self.ops[eng].append(dict(fn=fn, waits=waits, tok=tok))
        for k in writes:
            self.lastw[k] = tok
            self.readers[k] = []
        for k in reads:
            self.readers.setdefault(k, []).append(tok)
        return tok

    def barrier(self):
        if not self.enabled:
            return
        toks = []
        for e in self.ENG:
            n = len(self.ops[e])
            for i in range(n - 1, -1, -1):
                o = self.ops[e][i]
                if o['tok'][0] == 'c' and o['fn'] is not None:
                    toks.append(('c', e, i))
                    break
        for s in range(self.NDMA):
            if self.dma_cnt[s] > 0:
                toks.append(('d', s, self.dma_cnt[s]))
        for e in self.ENG:
            waits = []
            wd = self.waited[e]
            for t in toks:
                if t[0] == 'c' and t[1] == e and e == 'pe':
                    continue
                key = (t[0], t[1])
                if wd.get(key, -1) >= t[2]:
                    continue
                wd[key] = t[2]
                waits.append(t)
            if waits:
                self.ops[e].append(dict(fn=None, waits=waits, tok=('n', e, len(self.ops[e]))))

    def emit(self):
        nc = self.nc
        sig = {e: set() for e in self.ENG}
        for e in self.ENG:
            for o in self.ops[e]:
                for w in o['waits']:
                    if w[0] == 'c':
                        sig[w[1]].add(w[2])
        tick = {}
        nsem = {}
        for e in self.ENG:
            for r, i in enumerate(sorted(sig[e])):
                tick[(e, i)] = r + 1
            nsem[e] = (len(sig[e]) + self.SEG - 1) // self.SEG
        esem = {e: [self.es.enter_context(nc.semaphore("s_%s_%d" % (e, j))) for j in range(nsem[e])]
                for e in self.ENG}
        dsem = [self.es.enter_context(nc.semaphore("s_dma_%d" % j)) for j in range(self.NDMA)]
        SEG = self.SEG

        def semval(e, i):
            t = tick[(e, i)] - 1
            return esem[e][t // SEG], (t % SEG) + 1

        def run(e, eng):
            for i, o in enumerate(self.ops[e]):
                for w in o['waits']:
                    if w[0] == 'c':
                        s, v = semval(w[1], w[2])
                    else:
                        s, v = dsem[w[1]], w[2]
                    eng.wait_ge(s, v)
                if o['fn'] is None:
                    continue
                ins = o['fn'](eng)
                if o['tok'][0] == 'd':
                    ins.then_inc(dsem[o['tok'][1]], 16)
                elif i in sig[e]:
                    s, v = semval(e, i)
                    ins.then_inc(s, 1)

        with nc.Block() as block:
            @block.tensor
            def _(eng):
                run('pe', eng)

            @block.scalar
            def _(eng):
                run('act', eng)

            @block.vector
            def _(eng):
                run('dve', eng)

            @block.gpsimd
            def _(eng):
                run('pool', eng)

            @block.sync
            def _(eng):
                run('sp', eng)
        return {e: len(self.ops[e]) for e in self.ENG}


class Alloc:
    def __init__(self, arena, start, end):
        self.arena = arena
        self.off = start
        self.end = end

    def _take(self, nwords):
        self.off = (self.off + 15) // 16 * 16
        o = self.off
        self.off += nwords
        assert self.off <= self.end, ("arena overflow", self.off, self.end)
        return o

    def _shape(self, ap, shape):
        if len(shape) == 2:
            return ap
        if len(shape) == 3:
            return ap.rearrange("p (a b) -> p a b", a=shape[1])
        if len(shape) == 4:
            return ap.rearrange("p (a b c) -> p a b c", a=shape[1], b=shape[2])
        raise ValueError(shape)

    def f32(self, shape):
        n = int(np.prod(shape[1:]))
        o = self._take(n)
        return self._shape(self.arena[0:shape[0], o:o + n], shape)

    def i32(self, shape):
        n = int(np.prod(shape[1:]))
        o = self._take(n)
        return self._shape(self.arena[0:shape[0], o:o + n].bitcast(I32), shape)

    def bf16(self, shape):
        n = int(np.prod(shape[1:]))
        w = (n + 1) // 2
        o = self._take(w)
        ap = self.arena[0:shape[0], o:o + w].bitcast(BF16)
        if 2 * w != n:
            ap = ap[:, 0:n]
        return self._shape(ap, shape)


def build_program(debug=False, stop=None):
    nc = bass.Bass("TRN2", target_bir_lowering=False)

    def din(name, shape, dt=F32):
        return nc.dram_tensor(name, list(shape), dt, kind="ExternalInput").ap()

    skind = "ExternalOutput" if debug else "Internal"

    def dscr(name, shape, dt):
        return nc.dram_tensor(name, list(shape), dt, kind=skind).ap()

    x_d = din("x", [T, D])
    ctx_d = din("ctx", [TC, D])
    rows_d = din("rows", [NR, 128])
    w_ada_d = din("w_ada", [D, 6 * D])
    b_ada_d = din("b_ada", [1, 6 * D])
    w_in_d = din("w_in", [D, 2560])
    lru_wa_d = din("lru_wa", [16, 64, 64])
    lru_wx_d = din("lru_wx", [16, 64, 64])
    filt_w1_d = din("filt_w1", [33, 64])
    filt_cols_d = din("filt_cols", [64, 4])
    filt_w2_d = din("filt_w2", [64, 64])
    filt_w3_d = din("filt_w3", [64, 2048])
    filt_b3_d = din("filt_b3", [1, 2048])
    filt_bias_d = din("filt_bias", [1, 1024])
    w_out_d = din("w_out", [D, D])
    w_r_d = din("w_r", [D, 36])
    b_r_d = din("b_r", [1, 36])
    w_gate_d = din("w_gate", [NE, D, DE])
    w_up_d = din("w_up", [NE, D, DE])
    w_down_d = din("w_down", [NE, DE, D])
    final_g_d = din("final_g", [1, D])
    out_d = nc.dram_tensor("out", [T, D], F32, kind="ExternalOutput").ap()

    xpe_s = dscr("xpe_s", [NT, 128, D], F32)
    ya_s = dscr("ya_s", [4, 128, T], BF16)
    yb_s = dscr("yb_s", [4, 128, T], BF16)
    hy_s = dscr("hy_s", [3, 128, NT, 512], BF16)
    dftc_s = dscr("dftc_s", [16, 128, NT, 128], BF16)
    dfts_s = dscr("dfts_s", [16, 128, NT, 128], BF16)
    dbg_s = dscr("dbg_s", [8, 128, 2320], F32) if debug else None

    es = ExitStack()
    with es:
        fw = FW(nc, es)
        NW = 53200
        arena = es.enter_context(nc.sbuf_tensor("arena", [128, NW], F32))
        psum = es.enter_context(nc.psum_tensor("psum", [128, 8, 512], F32))
        st = dict(psrr=0)

        st['nb'] = 8

        def bank():
            b = st['psrr'] % st['nb']
            st['psrr'] = (b + 1) % st['nb']
            return b

        def chk(name):
            if stop == name:
                fw.barrier()
                fw.enabled = False

        def mm(out, lhsT, rhs, start, stop, r, w):
            fw.op('pe', lambda e: e.matmul(out, lhsT, rhs, start=start, stop=stop), r, w)

        def tr(out, in_, ident, r, w):
            fw.op('pe', lambda e: e.transpose(out, in_, ident), r, w)

        def act(out, in_, func, r, w, scale=None, bias=None, accum=None):
            kw = {}
            if scale is not None:
                kw['scale'] = scale
            if bias is not None:
                kw['bias'] = bias
            if accum is not None:
                kw['accum_out'] = accum
            fw.op('act', lambda e: e.activation(out=out, in_=in_, func=func, **kw), r, w)

        def ts(eng, out, in0, s1, s2, op0, op1, r, w):
            if s2 is None:
                fw.op(eng, lambda e: e.tensor_scalar(out=out, in0=in0, scalar1=s1, scalar2=None, op0=op0), r, w)
            else:
                fw.op(eng, lambda e: e.tensor_scalar(out=out, in0=in0, scalar1=s1, scalar2=s2, op0=op0, op1=op1), r, w)

        def stt(eng, out, in0, scalar, in1, op0, op1, r, w):
            fw.op(eng, lambda e: e.scalar_tensor_tensor(out=out, in0=in0, scalar=scalar, in1=in1, op0=op0, op1=op1), r, w)

        def tt(eng, out, in0, in1, op, r, w):
            fw.op(eng, lambda e: e.tensor_tensor(out=out, in0=in0, in1=in1, op=op), r, w)

        def cp(eng, out, in_, r, w):
            if eng == 'act':
                fw.op(eng, lambda e: e.activation(out=out, in_=in_, func=AF.Copy), r, w)
            else:
                fw.op(eng, lambda e: e.tensor_copy(out=out, in_=in_), r, w)

        def mset(eng, out, val, w):
            fw.op(eng, lambda e: e.memset(out, val), (), w)

        def dma(q, out, in_, r, w):
            fw.op(q, lambda e: e.dma_start(out=out, in_=in_), r, w, dma=True)

        def iota(out, pattern, base, cm, w):
            fw.op('pool', lambda e: e.iota(out, pattern, base=base, channel_multiplier=cm), (), w)

        P = Alloc(arena, 0, 4200)
        ident_f = P.f32([128, 128])
        ident_b = P.bf16([128, 128])
        ones_f = P.f32([128, 128])
        cst = P.f32([128, 8])
        cols = P.f32([128, NR])
        modc = P.f32([128, 32, 2])
        scl1 = P.f32([128, 8, 2])
        scl2 = P.f32([128, 8])
        g_rep = P.f32([128, 4, 512])
        ssa = P.f32([128, 4, 16])
        rstd_a = P.f32([128, 16])
        rstd_b = P.f32([128, 16])
        misc = P.f32([128, 64])
        C_NEGPI, C_ONE, C_EPS, C_ZERO = 0, 1, 2, 3

        A = Alloc(arena, P.end, NW)
        W = [A.f32([128, 2320]) for _ in range(8)]
        Wx = A.f32([128, 2320])
        amark = A.off
        E = A.f32([64, 512])
        selr = A.f32([64, 16, 128])
        selc = A.f32([64, 128])
        pec = A.f32([128, 512])
        NXT = 4
        xt = [A.f32([128, 1024]) for _ in range(NXT)]
        junk = A.bf16([128, 1024])
        Al = Alloc(arena, amark, A.off)
        W.append(Al.f32([128, 2320]))
        W.append(Al.f32([128, 2320]))
        W.append(Wx)
        A.off = (A.off + 15) // 16 * 16
        mark0 = A.off
        hT = A.bf16([128, 8, T])
        hcT = A.bf16([128, 8, TC])
        wib = [A.bf16([128, 8, 128]) for _ in range(3)]
        Wbd = A.f32([128, 16, 128])
        mark1 = A.off
        Wbdb = A.bf16([128, 16, 128])
        Vbp = [A.bf16([128, 2320]) for _ in range(2)]
        hcb = [A.bf16([128, T]) for _ in range(2)]
        hyst = arena[:, (P.end + 15) // 16 * 16 + 6 * 2320:(P.end + 15) // 16 * 16 + 6 * 2320 + 4096].bitcast(BF16).rearrange("p (a b) -> p a b", a=16)
        yast = hcb[0]
        halfb = A.f32([128, 16])
        spc = A.f32([128, 8, 4])
        tcol = A.f32([128, 16])
        tcol_i = A.i32([128, 16])
        ss1 = misc[:, 0:18]
        rs1 = misc[:, 18:36]
        rstd1 = misc[:, 36:54]

        def bank01():
            b = st.get('b01', 0)
            st['b01'] = 1 - b
            return b

        def pair():
            p = st.get('pair', 0)
            st['pair'] = (p + 1) % 3
            return 2 + 2 * p

        A0 = Alloc(arena, mark0, mark1)
        idi = A0.i32([128, 128])
        iota(idi, [[1, 128]], 0, -1, ['idi'])
        cp('dve', ident_f, idi, ['idi'], ['ident_f'])
        fw.op('dve', lambda e: e.tensor_single_scalar(out=ident_f, in_=ident_f, scalar=0.0, op=ALU.is_equal),
              ['ident_f'], ['ident_f'])
        cp('dve', ident_b, ident_f, ['ident_f'], ['ident_b'])
        mset('dve', ones_f, 1.0, ['ones_f'])
        mset('dve', cst[:, C_NEGPI:C_NEGPI + 1], -math.pi, ['cst'])
        mset('dve', cst[:, C_ONE:C_ONE + 1], 1.0, ['cst'])
        mset('dve', cst[:, C_EPS:C_EPS + 1], EPS, ['cst'])
        mset('dve', cst[:, C_ZERO:C_ZERO + 1], 0.0, ['cst'])
        negpi = cst[:, C_NEGPI:C_NEGPI + 1]
        one_c = cst[:, C_ONE:C_ONE + 1]
        eps_c = cst[:, C_EPS:C_EPS + 1]

        def sin_reduce(out, in_, np_, p0, off_turns, tA, tB, tI, r, w, tkey):
            ts('dve', tA, in_, 1.0 / TWO_PI, 16.5 + off_turns, ALU.mult, ALU.add, r, [tkey + 'A'])
            cp('dve', tI, tA, [tkey + 'A'], [tkey + 'I'])
            cp('dve', tB, tI, [tkey + 'I'], [tkey + 'B'])
            tt('dve', tA, tA, tB, ALU.subtract, [tkey + 'A', tkey + 'B'], [tkey + 'A'])
            stt('dve', tA, tA, 0.0, tA, ALU.is_lt, ALU.add, [tkey + 'A'], [tkey + 'A'])
            p0 = p0 or 0
            act(out, tA, AF.Sin, [tkey + 'A', 'cst'], w, scale=TWO_PI, bias=negpi[p0:p0 + np_, :])

        rows_sb = A0.f32([128, 2, 128])
        dma('sp', rows_sb[:, 0, :], rows_d[0:128, :], [], ['rows0'])
        dma('sp', rows_sb[0:NR - 128, 1, :], rows_d[128:NR, :], [], ['rows1'])
        b0 = bank()
        tr(psum[:, b0, 0:128], rows_sb[:, 0, :], ident_f, ['rows0', 'ident_f'], ['ps%d' % b0])
        tr(psum[:, b0, 128:NR], rows_sb[0:NR - 128, 1, :], ident_f[0:NR - 128, 0:NR - 128],
           ['rows1', 'ident_f'], ['ps%d' % b0])
        cp('dve', cols, psum[:, b0, 0:NR], ['ps%d' % b0], ['cols'])

        chk('const')
        sc = A0.f32([128, 8, 2])
        rep = A0.f32([128, 8, 128])
        bada_row = A0.f32([1, 2048])
        wa_buf = [A0.f32([128, 8, 256]) for _ in range(4)]
        act(sc[:, :, 0], cols[:, R_C:R_C + 8], AF.Silu, ['cols'], ['sc'])
        act(sc[:, :, 1], cols[:, R_CC:R_CC + 8], AF.Silu, ['cols', 'sc'], ['sc'])
        for k in range(8):
            ts('dve', rep[:, k, :], ones_f, sc[:, k, 0:1], None, ALU.mult, None, ['ones_f', 'sc'], ['rep'])
        dma('sp', bada_row[:, 0:1024], b_ada_d[:, 2048:3072], [], ['bada_row'])
        dma('sp', bada_row[:, 1024:2048], b_ada_d[:, 5120:6144], [], ['bada_row'])
        CB = [0, 1, 2, 3, 6, 7, 8, 9]
        w_ada_v = w_ada_d.rearrange("(k p) n -> p k n", p=128)

        def mod_load(hb):
            s = hb % 4
            dma('sp', wa_buf[s], w_ada_v[:, :, 256 * hb:256 * hb + 256], [], ['wa%d' % s])

        def mod_block(hb):
            s = hb % 4
            blk = hb // 2
            hf = hb % 2
            if blk in CB:
                jb = CB.index(blk)
                for c2 in range(2):
                    cc = 2 * hf + c2
                    b = bank()
                    for k in range(8):
                        mm(psum[:, b, 0:2], wa_buf[s][:, k, 128 * c2:128 * c2 + 128], sc[:, k, :],
                           k == 0, k == 7, ['wa%d' % s, 'sc'], ['ps%d' % b])
                    j = jb * 4 + cc
                    piece = R_BADA + blk * 4 + cc
                    ts('dve', modc[:, j, :], psum[:, b, 0:2], cols[:, piece:piece + 1], None, ALU.add, None,
                       ['ps%d' % b, 'cols'], ['modc'])
            else:
                gi = {4: 0, 5: 1, 10: 2, 11: 3}[blk]
                b = bank()
                for k in range(8):
                    mm(psum[:, b, 0:256], rep[:, k, :], wa_buf[s][:, k, :], k == 0, False,
                       ['wa%d' % s, 'rep'], ['ps%d' % b])
                mm(psum[:, b, 0:256], ones_f[0:1, :], bada_row[0:1, 512 * gi + 256 * hf:512 * gi + 256 * hf + 256],
                   False, True, ['ones_f', 'bada_row'], ['ps%d' % b])
                cp('act', g_rep[:, gi, 256 * hf:256 * hf + 256], psum[:, b, 0:256], ['ps%d' % b], ['g_rep'])

        kvec_i = W[1][:, 0:2048].bitcast(I32)
        kvec = W[0][:, 0:2048]
        iota(kvec_i, [[2, 2048]], 1, 0, ['W1'])
        cp('dve', kvec, kvec_i, ['W1'], ['W0'])
        iota(tcol_i, [[256, 16]], 1, 2, ['tcol_i'])
        cp('dve', tcol, tcol_i, ['tcol_i'], ['tcol'])
        PFi = W[1][:, 0:2048].bitcast(I32)
        MSi = W[2][:, 0:2048].bitcast(I32)
        PCi = W[3][:, 0:2048].bitcast(I32)
        PF3 = PFi.rearrange("p (i q) -> p i q", i=16)
        tcb = tcol.unsqueeze(2).to_broadcast([128, 16, 128])

        def dft_iter(j):
            kb_ = kvec[:, 128 * j:128 * j + 128].unsqueeze(1).to_broadcast([128, 16, 128])
            tt('dve', PF3, tcb, kb_, ALU.mult, ['W0', 'tcol'], ['W1'])
            fw.op('dve', lambda e: e.tensor_single_scalar(out=MSi, in_=PFi, scalar=16383, op=ALU.bitwise_and),
                  ['W1'], ['W2'])
            act(hcb[0], MSi, AF.Sin, ['W2', 'cst'], ['hcb0'], scale=TWO_PI / 16384.0, bias=negpi)
            dma('sp', dfts_s[j].rearrange("p i q -> p (i q)"), hcb[0], ['hcb0'], [('dfts', j)])
            fw.op('dve', lambda e: e.tensor_single_scalar(out=PCi, in_=PFi, scalar=4096, op=ALU.add),
                  ['W1'], ['W3'])
            fw.op('dve', lambda e: e.tensor_single_scalar(out=PCi, in_=PCi, scalar=16383, op=ALU.bitwise_and),
                  ['W3'], ['W3'])
            act(hcb[1], PCi, AF.Sin, ['W3', 'cst'], ['hcb1'], scale=TWO_PI / 16384.0, bias=negpi)
            dma('sp', dftc_s[j].rearrange("p i q -> p (i q)"), hcb[1], ['hcb1'], [('dftc', j)])

        for hb in range(3):
            mod_load(hb)
        for hb in range(24):
            if hb + 3 < 24:
                mod_load(hb + 3)
            mod_block(hb)
            if hb % 3 != 2:
                dft_iter(hb - hb // 3)
        for v in range(2):
            stt('dve', scl1[:, :, v], modc[:, 8:16, v], 1.0, cols[:, R_N1:R_N1 + 8], ALU.add, ALU.mult,
                ['modc', 'cols'], ['scl1'])
        stt('dve', scl2, modc[:, 24:32, 0], 1.0, cols[:, R_N2:R_N2 + 8], ALU.add, ALU.mult,
            ['modc', 'cols'], ['scl2'])
        fw.barrier()
        chk('mod')

        chk('dft')
        jrow_i = W[5][0:64, 0:256].bitcast(I32)
        jrow_f = W[4][0:64, 0:256]
        omega = W[4][0:64, 256:512]
        ncol_i = W[5][0:64, 256:257].bitcast(I32)
        ncol_f = W[4][0:64, 512:513]
        ang = W[4][0:64, 768:1024]
        iota(jrow_i, [[1, 256]], 0, 0, ['W5'])
        cp('dve', jrow_f, jrow_i, ['W5'], ['W4'])
        act(omega, jrow_f, AF.Exp, ['W4'], ['W4'], scale=-math.log(10000.0) / 256.0)
        iota(ncol_i, [[0, 1]], 0, 1, ['W5'])
        cp('dve', ncol_f, ncol_i, ['W5'], ['W4'])
        ts('dve', ang, omega, ncol_f, None, ALU.mult, None, ['W4'], ['W4'])
        tA = W[4][0:64, 1024:1280]
        tB = W[4][0:64, 1280:1536]
        tI = W[5][0:64, 512:768].bitcast(I32)
        sin_reduce(E[:, 0:256], ang, 64, None, 0.0, tA, tB, tI, ['W4'], ['E'], 'srt')
        sin_reduce(E[:, 256:512], ang, 64, None, 0.25, tA, tB, tI, ['W4'], ['E'], 'srt')
        selc_i = W[5][0:64, 1024:1152].bitcast(I32)
        iota(selc_i.rearrange("p (a b) -> p a b", a=2), [[0, 2], [1, 64]], 0, -1, ['W5'])
        cp('dve', selc, selc_i, ['W5'], ['selc'])
        fw.op('dve', lambda e: e.tensor_single_scalar(out=selc, in_=selc, scalar=0.0, op=ALU.is_equal),
              ['selc'], ['selc'])
        selr_i = W[3][0:64, 0:2048].bitcast(I32)
        iota(selr_i.rearrange("p (a b c) -> p a b c", a=16, b=2), [[2, 16], [1, 2], [0, 64]], 0, -1, ['W3'])
        selr_flat = selr.rearrange("p a b -> p (a b)")
        cp('dve', selr_flat, selr_i, ['W3'], ['selr'])
        fw.op('dve', lambda e: e.tensor_single_scalar(out=selr_flat, in_=selr_flat, scalar=0.0, op=ALU.is_equal),
              ['selr'], ['selr'])
        b = bank01()
        mm(psum[:, b, :], selc, E, True, True, ['selc', 'E'], ['ps%d' % b])
        cp('act', pec, psum[:, b, :], ['ps%d' % b], ['pec'])
        mset('dve', ss1, 0.0, ['ss1'])

        chk('pe')
        mset('dve', Wbd, 0.0, ['Wbd'])
        for dd in range(2):
            for g, src in enumerate((lru_wa_d, lru_wx_d)):
                for c in range(4):
                    slot = (dd * 2 + g) * 4 + c
                    dma('pool', Wbd[0:64, slot, 0:64], src[dd * 8 + 2 * c], ['Wbd'], [('Wbdd', slot)])
                    dma('pool', Wbd[64:128, slot, 64:128], src[dd * 8 + 2 * c + 1], ['Wbd'], [('Wbdd', slot)])
        ts('dve', halfb[:, 0:16], cols[:, R_BA:R_BA + 16], 0.5, None, ALU.mult, None, ['cols'], ['halfb'])
        lamc = cols[:, R_LAM:R_LAM + 8]
        stt('dve', spc[:, :, 2], lamc, -1.0, lamc, ALU.mult, ALU.max, ['cols'], ['spc'])
        act(spc[:, :, 2], spc[:, :, 2], AF.Exp, ['spc'], ['spc'], scale=-1.0)
        act(spc[:, :, 2], spc[:, :, 2], AF.Ln, ['spc', 'cst'], ['spc'], scale=1.0, bias=one_c)
        ts('dve', spc[:, :, 3], lamc, -1.0, 0.0, ALU.mult, ALU.max, ['cols', 'spc'], ['spc'])
        tt('dve', spc[:, :, 2], spc[:, :, 2], spc[:, :, 3], ALU.add, ['spc'], ['spc'])
        ts('dve', spc[:, :, 0], spc[:, :, 2], -4.0, None, ALU.mult, None, ['spc'], ['spc'])
        ts('dve', spc[:, :, 1], spc[:, :, 2], -8.0, None, ALU.mult, None, ['spc'], ['spc'])

        def p1_stage0(ii):
            s = ii % NXT
            lat = ii < NT
            xk = 'xt%d' % s
            src = x_d[128 * ii:128 * ii + 128, :] if lat else ctx_d[128 * (ii - NT):128 * (ii - NT) + 128, :]
            dma('sp', xt[s], src, [], [xk])

        def p1_stage1(ii):
            s = ii % NXT
            lat = ii < NT
            xk = 'xt%d' % s
            if lat:
                b = bank01()
                mm(psum[:, b, :], selr[:, ii, :], E, True, True, ['selr', 'E'], ['ps%d' % b])
                tt('dve', xt[s][:, 0:512], xt[s][:, 0:512], psum[:, b, :], ALU.add, [xk, 'ps%d' % b], [xk])
                tt('dve', xt[s][:, 512:1024], xt[s][:, 512:1024], pec, ALU.add, [xk, 'pec'], [xk])
                dma('sp', xpe_s[ii], xt[s], [xk], [('xpe', ii)])
            act(junk, xt[s], AF.Square, [xk], ['junk', 'ss1'], accum=ss1[:, ii:ii + 1])
            act(rs1[:, ii:ii + 1], ss1[:, ii:ii + 1], AF.Sqrt, ['ss1', 'cst'], ['rs1'], scale=1.0 / D, bias=eps_c)
            fw.op('dve', lambda e, ii=ii: e.reciprocal(out=rstd1[:, ii:ii + 1], in_=rs1[:, ii:ii + 1]),
                  ['rs1'], ['rstd1'])
            ts('dve', xt[s], xt[s], rstd1[:, ii:ii + 1], None, ALU.mult, None, [xk, 'rstd1'], [xk])

        def p1_stage2(ii):
            s = ii % NXT
            lat = ii < NT
            v = 0 if lat else 1
            xk = 'xt%d' % s
            pb = pair()
            pk = ['ps%d' % pb, 'ps%d' % (pb + 1)]
            psT = psum[:, pb:pb + 2, :].rearrange("p a (b c) -> p (a b) c", c=128)
            for k in range(8):
                tr(psT[:, k, :], xt[s][:, 128 * k:128 * k + 128], ident_f, [xk, 'ident_f'], pk)
            for k in range(8):
                if lat:
                    dest = hT[:, k, 128 * ii:128 * ii + 128]
                else:
                    dest = hcT[:, k, 128 * (ii - NT):128 * (ii - NT) + 128]
                if k < 4:
                    act(dest, psT[:, k, :], AF.Identity, [pk[0], 'scl1', 'modc'], [('hT', ii)],
                        scale=scl1[:, k, v:v + 1], bias=modc[:, k, v:v + 1])
                else:
                    ts('dve', dest, psT[:, k, :], scl1[:, k, v:v + 1], modc[:, k, v:v + 1], ALU.mult, ALU.add,
                       [pk[1], 'scl1', 'modc'], [('hT', ii)])

        NP1T = NT + 2
        p1_stage0(0)
        p1_stage0(1)
        p1_stage0(2)
        p1_stage1(0)
        p1_stage1(1)
        for ii in range(NP1T):
            if ii + 3 < NP1T:
                p1_stage0(ii + 3)
            if ii + 2 < NP1T:
                p1_stage1(ii + 2)
            p1_stage2(ii)

        chk('p1')
        w_in_v = w_in_d.rearrange("(k p) n -> p k n", p=128)
        order = []
        for c in range(4):
            order += [c, 4 + c]
        order += list(range(8, 20))
        st['wslot'] = {}

        def load_win(pos):
            if pos >= len(order):
                return
            cc = order[pos]
            s = pos % 3
            st['wslot'][cc] = s
            dma('pool', wib[s], w_in_v[:, :, 128 * cc:128 * cc + 128], [], ['wib%d' % s])

        load_win(0)
        load_win(1)
        hkeys = [[('hT', 4 * n + j) for j in range(4)] for n in range(4)]
        ckeys = [('hT', NT), ('hT', NT + 1)]

        def proj(cc, evac, evac_ctx=None):
            s = st['wslot'][cc]
            wk = 'wib%d' % s
            for n in range(4):
                b = bank()
                for k in range(8):
                    mm(psum[:, b, :], wib[s][:, k, :], hT[:, k, 512 * n:512 * n + 512], k == 0, k == 7,
                       [wk] + hkeys[n], ['ps%d' % b])
                evac(n, b)
            if evac_ctx is not None:
                b = bank()
                for k in range(8):
                    mm(psum[:, b, 0:TC], wib[s][:, k, :], hcT[:, k, :], k == 0, k == 7, [wk] + ckeys, ['ps%d' % b])
                evac_ctx(b)

        st['pos'] = 0
        NV = 2307
        UXp, Vp, GAp = [W[0], W[8]], [W[1], W[9]], [W[2], W[10]]
        UXk, Vk, GAk = ['W0', 'W8'], ['W1', 'W9'], ['W2', 'W10']
        blocks = [(0, 512), (512, 1024), (1024, 1536), (1536, 2048), (2048, NV)]

        def rnn_stageA(c):
            par = c % 2
            U, V, GA, Vb = UXp[par], Vp[par], GAp[par], Vbp[par]
            uk, vk, gk, vbk = UXk[par], Vk[par], GAk[par], 'Vb%d' % par
            load_win(st['pos'] + 2)

            def ev_ga(n, b):
                act(GA[:, 512 * n:512 * n + 512], psum[:, b, :], AF.Gelu_apprx_tanh, ['ps%d' % b], [gk])
            proj(c, ev_ga)
            st['pos'] += 1
            load_win(st['pos'] + 2)
            mset('dve', U[:, 0:2], 0.0, [uk])
            mset('dve', U[:, 258:261], 0.0, [uk])
            mset('dve', U[:, 2309:2310], 0.0, [uk])

            def ev_u(n, b):
                eng = 'dve' if n % 2 == 0 else 'act'
                cp(eng, U[:, 261 + 512 * n:261 + 512 * n + 512], psum[:, b, :], ['ps%d' % b], [uk])

            def ev_uc(b):
                cp('dve', U[:, 2:2 + TC], psum[:, b, 0:TC], ['ps%d' % b], [uk])
            proj(4 + c, ev_u, ev_uc)
            st['pos'] += 1
            wc = [cols[:, R_CAW + 4 * k + c:R_CAW + 4 * k + c + 1] for k in range(4)]
            cb = cols[:, R_CAB + c:R_CAB + c + 1]
            ts('dve', V[:, 0:NV], U[:, 0:NV], wc[0], cb, ALU.mult, ALU.add, [uk, 'cols'], [vk])
            for k in range(1, 4):
                stt('dve', V[:, 0:NV], U[:, k:k + NV], wc[k], V[:, 0:NV], ALU.mult, ALU.add,
                    [uk, vk, 'cols'], [vk])
            cp('dve', Vb[:, 0:NV], V[:, 0:NV], [vk], [vbk])

        def rnn_stageB(c):
            par = c % 2
            V, GA, Vb = Vp[par], GAp[par], Vbp[par]
            XB, XF = UXp[par], W[5]
            uk, vk, gk, vbk = UXk[par], Vk[par], GAk[par], 'Vb%d' % par
            for dd in range(2):
                X = XF if dd == 0 else XB
                XK = 'W5' if dd == 0 else uk
                TRb, TIb = (W[3], W[4]) if dd == 0 else (W[6], W[7])
                TRK, TIK = ('W3', 'W4') if dd == 0 else ('W6', 'W7')
                dc = dd * 4 + c
                for g, (dst, dk) in enumerate(((TRb, TRK), (TIb, TIK))):
                    slot = (dd * 2 + g) * 4 + c
                    hb = halfb[:, g * 8 + dc:g * 8 + dc + 1]
                    for (lo, hi) in blocks:
                        b = bank()
                        mm(psum[:, b, 0:hi - lo], Wbdb[:, slot, :], Vb[:, lo:hi], True, True,
                           ['Wbdb', vbk], ['ps%d' % b])
                        act(dst[:, lo:hi], psum[:, b, 0:hi - lo], AF.Tanh, ['ps%d' % b, 'halfb'], [dk],
                            scale=0.5, bias=hb)
                m4 = spc[:, dc, 0:1]
                m8 = spc[:, dc, 1:2]
                act(X[:, 0:NV], TRb[:, 0:NV], AF.Exp, [TRK, 'spc'], [XK], scale=m8, bias=m8)
                act(TRb[:, 0:NV], TRb[:, 0:NV], AF.Exp, [TRK, 'spc'], [TRK], scale=m4, bias=m4)
                act(X[:, 0:NV], X[:, 0:NV], AF.Sqrt, [XK, 'cst'], [XK], scale=-1.0, bias=one_c)
                stt('dve', TIb[:, 0:NV], TIb[:, 0:NV], 1.0, V[:, 0:NV], ALU.add, ALU.mult, [TIK, vk], [TIK])
                stt('dve', TIb[:, 0:NV], TIb[:, 0:NV], 0.5, X[:, 0:NV], ALU.mult, ALU.mult, [TIK, XK], [TIK])
                if dd == 0:
                    fw.op('dve', lambda e, X=X, TRb=TRb, TIb=TIb: e.tensor_tensor_scan(
                        out=X[:, 0:TC], data0=TRb[:, 0:TC], data1=TIb[:, 0:TC], initial=0.0,
                        op0=ALU.mult, op1=ALU.add), [TRK, TIK, XK], [XK])
                    fw.op('dve', lambda e, X=X, TRb=TRb, TIb=TIb: e.tensor_tensor_scan(
                        out=X[:, 259:NV], data0=TRb[:, 259:NV], data1=TIb[:, 259:NV], initial=X[:, TC - 1:TC],
                        op0=ALU.mult, op1=ALU.add), [TRK, TIK, XK], [XK])
                else:
                    fw.op('dve', lambda e, X=X, TRb=TRb, TIb=TIb: e.tensor_tensor_scan(
                        out=X[:, 0:TC][:, ::-1], data0=TRb[:, 0:TC][:, ::-1], data1=TIb[:, 0:TC][:, ::-1],
                        initial=0.0, op0=ALU.mult, op1=ALU.add), [TRK, TIK, XK], [XK])
                    fw.op('dve', lambda e, X=X, TRb=TRb, TIb=TIb: e.tensor_tensor_scan(
                        out=X[:, 259:NV][:, ::-1], data0=TRb[:, 259:NV][:, ::-1], data1=TIb[:, 259:NV][:, ::-1],
                        initial=X[:, 0:1], op0=ALU.mult, op1=ALU.add), [TRK, TIK, XK], [XK])
            L0, L1 = 259, NV
            tt('dve', XF[:, L0:L1], XF[:, L0:L1], XB[:, L0:L1], ALU.add, ['W5', uk], ['W5'])
            tt('dve', XF[:, L0:L1], XF[:, L0:L1], GA[:, 0:T], ALU.mult, ['W5', gk], ['W5'])
            SQ = W[3]
            act(SQ[:, 0:T], XF[:, L0:L1], AF.Square, ['W5'], ['W3'])
            ts('dve', yast, XF[:, L0:L1], cols[:, R_ONA + c:R_ONA + c + 1], None, ALU.mult, None,
               ['W5', 'cols'], ['hcb0'])
            dma('sp', ya_s[c], yast, ['hcb0'], [('ya_s', c)])

        def rnn_stageC(c):
            SQ = W[3]
            b = bank()
            for i in range(NT):
                mm(psum[:, b, i:i + 1], SQ[:, 128 * i:128 * i + 128], ones_f[:, 0:1], True, True,
                   ['W3', 'ones_f'], ['ps%d' % b])
            cp('dve', ssa[:, c, :], psum[:, b, 0:NT], ['ps%d' % b], ['ssa'])

        cp('dve', Wbdb, Wbd, ['Wbd'] + [('Wbdd', sl_) for sl_ in range(16)], ['Wbdb'])
        rnn_stageA(0)
        rnn_stageA(1)
        for c in range(4):
            rnn_stageB(c)
            if c + 2 < 4:
                rnn_stageA(c + 2)
            rnn_stageC(c)
        pos = st['pos']

        chk('rnn')
        for q in range(2):
            mset('dve', W[2 * q][:, 0:1], 0.0, ['W%d' % (2 * q)])
            mset('dve', W[2 * q][:, 2049:2050], 0.0, ['W%d' % (2 * q)])

        def hy_stageA(cc):
            q = cc % 2
            HY = W[2 * q]
            hk_ = 'W%d' % (2 * q)
            load_win(st['pos'] + 2)

            def ev_h(n, b):
                eng = 'dve' if n % 2 == 0 else 'act'
                cp(eng, HY[:, 1 + 512 * n:1 + 512 * n + 512], psum[:, b, :], ['ps%d' % b], [hk_])
            proj(cc, ev_h)
            st['pos'] += 1

        def hy_stageB(cc):
            g = (cc - 8) // 4
            c4 = (cc - 8) % 4
            q = cc % 2
            HY = W[2 * q]
            TMP = W[2 * q + 1]
            hk_ = 'W%d' % (2 * q)
            tk_ = 'W%d' % (2 * q + 1)
            wc = [cols[:, R_CBW + 12 * k + (cc - 8):R_CBW + 12 * k + (cc - 8) + 1] for k in range(3)]
            s = cc % 2
            ts('dve', TMP[:, 0:T], HY[:, 0:T], wc[0], None, ALU.mult, None, [hk_, 'cols'], [tk_])
            stt('dve', TMP[:, 0:T], HY[:, 1:1 + T], wc[1], TMP[:, 0:T], ALU.mult, ALU.add, [hk_, tk_, 'cols'], [tk_])
            stt('dve', hcb[s], HY[:, 2:2 + T], wc[2], TMP[:, 0:T], ALU.mult, ALU.add, [hk_, tk_, 'cols'],
                ['hcb%d' % s])
            for h in range(2):
                b = bank()
                psb = psum[:, b, :].bitcast(BF16)
                for i8 in range(8):
                    i = 8 * h + i8
                    tr(psb[:, 128 * i8:128 * i8 + 128], hcb[s][:, 128 * i:128 * i + 128], ident_b,
                       ['hcb%d' % s, 'ident_b'], ['ps%d' % b])
                cp('act' if h == 0 else 'dve', hyst[:, 8 * h:8 * h + 8, 128 * c4:128 * c4 + 128],
                   psb.rearrange("p (a b) -> p a b", a=8), ['ps%d' % b], ['hyst', 'W6', 'W7'])
            if c4 == 3:
                dma('sp', hy_s[g], hyst, ['hyst'], [('hy_s', g)])

        st['pos'] = pos
        hy_stageA(8)
        for cc in range(8, 20):
            if cc + 1 < 20:
                hy_stageA(cc + 1)
            hy_stageB(cc)
        fw.barrier()


        chk('A')
        Bq = Alloc(arena, P.end, NW)
        NS = 3
        cbuf = [Bq.bf16([128, 16, 128]) for _ in range(NS)]
        sbuf = [Bq.bf16([128, 16, 128]) for _ in range(NS)]
        NP1 = T + 1
        hdn2 = Bq.f32([65, 2052])
        w3aug = Bq.f32([65, 2048])
        w1t = Bq.f32([65, 64])
        w2t = Bq.f32([64, 64])
        fcols = Bq.f32([64, 4])
        delta = Bq.f32([128, 512])
        tsc = Bq.f32([128, 16, 2])
        psic = Bq.f32([128, 16, 3])
        smalli = Bq.i32([128, 16])
        fcol = Bq.f32([65, 2])
        fb_rep = Bq.f32([128, 2, 512])
        rnorm = Bq.f32([128, 512])
        dec = [Bq.f32([128, 512]) for _ in range(4)]
        kfb = [Bq.f32([128, 512]) for _ in range(4)]
        absum = [Bq.f32([128, 512]) for _ in range(2)]
        et = [Bq.f32([128, 512]) for _ in range(4)]
        xg = [Bq.bf16([128, 512]) for _ in range(2)]
        ymark = Bq.off
        SUM = Bq.bf16([128, 16, 512])
        DIF = Bq.bf16([128, 16, 512])
        Ut = Bq.bf16([128, 16, 512])
        Z1t = Bq.bf16([128, 16, 512])
        ybst = [SUM.rearrange("p a b -> p (a b)")[:, 0:T]]
        YA = Bq.bf16([128, 16, 512])
        YB = Bq.bf16([128, 16, 512])
        ssb = misc[:, 0:16]
        rsb = misc[:, 16:32]
        M = Alloc(arena, ymark, NW)
        zT = M.f32([65, 2052])
        h1 = M.f32([64, 2052])
        argb = M.f32([65, 2052])
        mtA = M.f32([65, 2052])
        mtB = M.f32([65, 2052])
        mtI = M.i32([65, 2052])
        posi = mtI

        dma('sp', fb_rep.rearrange("p a b -> p (a b)"), filt_bias_d.partition_broadcast(128), [], ['fb_rep'])
        mset('dve', w1t, 0.0, ['w1t'])
        dma('sp', w1t[0:16, :], filt_w1_d[1:17, :], ['w1t'], ['w1t_a'])
        dma('sp', w1t[32:48, :], filt_w1_d[17:33, :], ['w1t'], ['w1t_b'])
        dma('sp', w1t[64:65, :], filt_w1_d[0:1, :], ['w1t'], ['w1t_c'])
        dma('sp', w2t, filt_w2_d[:, :], [], ['w2t'])
        dma('sp', fcols, filt_cols_d[:, :], [], ['fcols'])
        dma('sp', w3aug[0:64, :], filt_w3_d[:, :], [], ['w3a'])
        dma('sp', w3aug[64:65, :], filt_b3_d[:, :], [], ['w3b'])

        n_ = float(T)
        iota(posi[:, 0:NP1], [[1, NP1]], 0, 0, ['mtI'])
        cp('dve', mtB[:, 0:NP1], posi[:, 0:NP1], ['mtI'], ['mtB'])
        iota(smalli[0:65, 0:1], [[0, 1]], 0, 1, ['smalli'])
        fw.op('dve', lambda e: e.tensor_single_scalar(out=smalli[0:65, 1:2], in_=smalli[0:65, 0:1], scalar=31,
                                                      op=ALU.bitwise_and), ['smalli'], ['smalli'])
        cp('dve', fcol[:, 0:1], smalli[0:65, 1:2], ['smalli'], ['fcol'])
        bstep = (15.0 - 1e-4) / 15.0
        ts('dve', fcol[:, 1:2], fcol[:, 0:1], bstep * TWO_PI / n_, 1e-4 * TWO_PI / n_, ALU.mult, ALU.add,
           ['fcol'], ['fcol'])
        ts('dve', argb[0:64, 0:NP1], mtB[0:64, 0:NP1], fcol[0:64, 1:2], None, ALU.mult, None,
           ['mtB', 'fcol'], ['argb'])
        mset('dve', zT[:, 0:NP1], 0.0, ['zT'])
        sin_reduce(zT[0:32, 0:NP1], argb[0:32, 0:NP1], 32, None, 0.25, mtA[0:32, 0:NP1], h1[0:32, 0:NP1],
                   mtI[0:32, 0:NP1], ['argb', 'zT'], ['zT'], 'mz0')
        sin_reduce(zT[32:64, 0:NP1], argb[32:64, 0:NP1], 32, 32, 0.5, mtA[32:64, 0:NP1], h1[32:64, 0:NP1],
                   mtI[32:64, 0:NP1], ['argb', 'zT'], ['zT'], 'mz1')
        ts('dve', zT[64:65, 0:NP1], mtB[64:65, 0:NP1], 1.0 / (n_ - 1.0), None, ALU.mult, None, ['mtB', 'zT'], ['zT'])
        pblocks = [(0, 512), (512, 1024), (1024, 1536), (1536, 2048), (2048, NP1)]
        for (lo, hi) in pblocks:
            b = bank()
            mm(psum[0:64, b, 0:hi - lo], w1t, zT[:, lo:hi], True, True,
               ['w1t', 'w1t_a', 'w1t_b', 'w1t_c', 'zT'], ['ps%d' % b])
            ts('dve', argb[0:64, lo:hi], psum[0:64, b, 0:hi - lo], fcols[:, 0:1], fcols[:, 1:2], ALU.add, ALU.mult,
               ['ps%d' % b, 'fcols'], ['argb'])
        sin_reduce(h1[:, 0:NP1], argb[0:64, 0:NP1], 64, None, 0.0, mtA[0:64, 0:NP1], zT[0:64, 0:NP1],
                   mtI[0:64, 0:NP1], ['argb', 'zT'], ['h1'], 'mz2')
        for (lo, hi) in pblocks:
            b = bank()
            mm(psum[0:64, b, 0:hi - lo], w2t, h1[:, lo:hi], True, True, ['w2t', 'h1'], ['ps%d' % b])
            ts('dve', argb[0:64, lo:hi], psum[0:64, b, 0:hi - lo], fcols[:, 2:3], fcols[:, 3:4], ALU.add, ALU.mult,
               ['ps%d' % b, 'fcols'], ['argb'])
        sin_reduce(hdn2[0:64, 0:NP1], argb[0:64, 0:NP1], 64, None, 0.0, mtA[0:64, 0:NP1], zT[0:64, 0:NP1],
                   mtI[0:64, 0:NP1], ['argb', 'h1'], ['hdn2'], 'mz3')
        mset('dve', hdn2[64:65, 0:NP1], 1.0, ['hdn2'])
        mset('dve', hdn2[:, T:NP1], 0.0, ['hdn2'])
        dma('sp', Ut, hy_s[0], [('hy_s', 0), 'hdn2'], ['Ut'])

        chk('mlp')
        dstep_lo = abs(math.log(1e-2) / 1.5)
        dstep_hi = abs(math.log(1e-2) / 0.3)
        di = et[0].bitcast(I32)
        iota(di, [[1, 512]], 0, 0, ['et0'])
        cp('dve', delta, di, ['et0'], ['delta'])
        ts('dve', delta, delta, (dstep_hi - dstep_lo) / 511.0, dstep_lo, ALU.mult, ALU.add, ['delta'], ['delta'])
        iota(smalli, [[128, 16]], 0, 1, ['smalli'])
        cp('dve', tsc[:, :, 0], smalli, ['smalli'], ['tsc'])
        ts('dve', tsc[:, :, 1], tsc[:, :, 0], 1.0, -1.0 / (n_ - 1.0), ALU.add, ALU.mult, ['tsc'], ['tsc'])
        ts('dve', tsc[:, :, 0], tsc[:, :, 0], -1.0 / (n_ - 1.0), None, ALU.mult, None, ['tsc'], ['tsc'])
        iota(smalli, [[256, 16]], 1, 2, ['smalli'])
        act(psic[:, :, 1], smalli, AF.Sin, ['smalli', 'cst'], ['psic'], scale=TWO_PI / 16384.0, bias=negpi)
        fw.op('dve', lambda e: e.tensor_single_scalar(out=smalli, in_=smalli, scalar=4096, op=ALU.add),
              ['smalli', 'psic'], ['smalli'])
        act(psic[:, :, 0], smalli, AF.Sin, ['smalli', 'cst'], ['psic'], scale=TWO_PI / 16384.0, bias=negpi)
        ts('dve', psic[:, :, 2], psic[:, :, 0], -1.0, None, ALU.mult, None, ['psic'], ['psic'])
        mset('dve', ssb, 0.0, ['ssb'])

        st['dpos'] = 0

        def load_dft(j):
            s = st['dpos'] % NS
            st['dpos'] += 1
            dma('sp', cbuf[s], dftc_s[j], [('dftc', j)], ['cbuf%d' % s])
            dma('sp', sbuf[s], dfts_s[j], [('dfts', j)], ['sbuf%d' % s])
            return s

        for o in range(2):
            Uin = Ut if o == 0 else Z1t
            UK = 'Ut' if o == 0 else 'Z1t'
            st['nb'] = 7
            st['psrr'] = 0
            for i in range(NT):
                b1 = bank()
                mm(psum[:, b1, :], hdn2[:, 128 * i:128 * i + 128], w3aug[:, 512 * o:512 * o + 512], True, True,
                   ['hdn2', 'w3a', 'w3b'], ['ps%d' % b1])
                b2 = bank()
                mm(psum[:, b2, :], hdn2[:, 128 * i + 1:128 * i + 129], w3aug[:, 1024 + 512 * o:1024 + 512 * o + 512],
                   True, True, ['hdn2', 'w3a', 'w3b'], ['ps%d' % b2])
                q = i % 2
                d0, d1, k0, k1, ab = dec[2 * q], dec[2 * q + 1], kfb[2 * q], kfb[2 * q + 1], absum[q]
                kd0, kd1, kk0, kk1, kab = 'dec%d' % (2 * q), 'dec%d' % (2 * q + 1), 'kf%d' % q, 'kb%d' % q, 'absum%d' % q
                act(d0, delta, AF.Exp, ['delta', 'tsc'], [kd0], scale=tsc[:, i, 0:1])
                act(d1, delta, AF.Exp, ['delta', 'tsc'], [kd1], scale=tsc[:, i, 1:2])
                tt('dve', k0, psum[:, b1, :], d0, ALU.mult, ['ps%d' % b1, kd0], [kk0])
                tt('dve', k1, psum[:, b2, :], d1, ALU.mult, ['ps%d' % b2, kd1], [kk1])
                tt('dve', SUM[:, i, :], k0, k1, ALU.add, [kk0, kk1], [('SUM', i)])
                tt('dve', DIF[:, i, :], k0, k1, ALU.subtract, [kk0, kk1], [('DIF', i)])
                stt('dve', k0, k0, -1.0, k0, ALU.mult, ALU.max, [kk0], [kk0])
                stt('dve', k1, k1, -1.0, k1, ALU.mult, ALU.max, [kk1], [kk1])
                tt('dve', ab, k0, k1, ALU.add, [kk0, kk1], [kab])
                if i > 0:
                    qp = (i - 1) % 2
                    mm(psum[:, 7, :], ones_f, absum[qp], i == 1, False, ['ones_f', 'absum%d' % qp], ['ps7'])
            mm(psum[:, 7, :], ones_f, absum[(NT - 1) % 2], False, True, ['ones_f', 'absum%d' % ((NT - 1) % 2)], ['ps7'])
            fw.op('dve', lambda e: e.reciprocal(out=rnorm, in_=psum[:, 7, :]), ['ps7'], ['rnorm'])
            st['nb'] = 8
            sl = {}
            sl[0] = load_dft(0)
            sl[1] = load_dft(1)
            SK = [('SUM', i) for i in range(NT)]
            DK = [('DIF', i) for i in range(NT)]
            for j in range(NT):
                if j + 2 < NT:
                    sl[j + 2] = load_dft(j + 2)
                s = sl[j]
                bGA, bGB, bA, bB = bank(), bank(), bank(), bank()
                for i in range(NT):
                    mm(psum[:, bGA, :], cbuf[s][:, i, :], SUM[:, i, :], i == 0, i == NT - 1,
                       ['cbuf%d' % s] + SK, ['ps%d' % bGA])
                    mm(psum[:, bA, :], cbuf[s][:, i, :], Uin[:, i, :], i == 0, i == NT - 1,
                       ['cbuf%d' % s, UK], ['ps%d' % bA])
                for i in range(NT):
                    mm(psum[:, bGB, :], sbuf[s][:, i, :], DIF[:, i, :], i == 0, i == NT - 1,
                       ['sbuf%d' % s] + DK, ['ps%d' % bGB])
                    mm(psum[:, bB, :], sbuf[s][:, i, :], Uin[:, i, :], i == 0, i == NT - 1,
                       ['sbuf%d' % s, UK], ['ps%d' % bB])
                ncos = psic[:, j, 0:1]
                nsin = psic[:, j, 1:2]
                pcos = psic[:, j, 2:3]
                kGA, kGB, kA, kB = 'ps%d' % bGA, 'ps%d' % bGB, 'ps%d' % bA, 'ps%d' % bB
                act(et[0], psum[:, bGA, :], AF.Copy, [kGA, 'psic'], ['et0'], scale=ncos)
                stt('dve', et[0], psum[:, bGB, :], nsin, et[0], ALU.mult, ALU.add, [kGB, 'psic', 'et0'], ['et0'])
                tt('dve', et[0], et[0], rnorm, ALU.mult, ['et0', 'rnorm'], ['et0'])
                act(et[1], psum[:, bGA, :], AF.Copy, [kGA, 'psic'], ['et1'], scale=nsin)
                stt('dve', et[1], psum[:, bGB, :], pcos, et[1], ALU.mult, ALU.add, [kGB, 'psic', 'et1'], ['et1'])
                tt('dve', et[1], et[1], rnorm, ALU.mult, ['et1', 'rnorm'], ['et1'])
                tt('dve', et[2], psum[:, bA, :], et[0], ALU.mult, [kA, 'et0'], ['et2'])
                tt('dve', et[3], psum[:, bB, :], et[1], ALU.mult, [kB, 'et1'], ['et3'])
                tt('dve', YA[:, j, :], et[2], et[3], ALU.add, ['et2', 'et3'], [('YA', j)])
                tt('dve', et[2], psum[:, bB, :], et[0], ALU.mult, [kB, 'et0', 'et2'], ['et2'])
                tt('dve', et[3], psum[:, bA, :], et[1], ALU.mult, [kA, 'et1', 'et3'], ['et3'])
                tt('dve', YB[:, j, :], et[2], et[3], ALU.subtract, ['et2', 'et3'], [('YB', j)])
            YAK = [('YA', i) for i in range(NT)]
            YBK = [('YB', i) for i in range(NT)]
            sl = {}
            sl[0] = load_dft(0)
            sl[1] = load_dft(1)
            for j in range(NT):
                if j + 2 < NT:
                    sl[j + 2] = load_dft(j + 2)
                s = sl[j]
                g2 = j % 2
                dma('sp', xg[g2], hy_s[1 + o][:, j, :], [('hy_s', 1 + o)], ['xg%d' % g2])
                b = bank()
                for i in range(NT):
                    mm(psum[:, b, :], cbuf[s][:, i, :], YA[:, i, :], i == 0, False, ['cbuf%d' % s] + YAK, ['ps%d' % b])
                for i in range(NT):
                    mm(psum[:, b, :], sbuf[s][:, i, :], YB[:, i, :], False, i == NT - 1, ['sbuf%d' % s] + YBK,
                       ['ps%d' % b])
                tt('dve', et[0], Uin[:, j, :], fb_rep[:, o, :], ALU.mult, [UK, 'fb_rep'], ['et0'])
                stt('dve', et[0], psum[:, b, :], 2.0 / 4096.0, et[0], ALU.mult, ALU.add, ['ps%d' % b, 'et0'], ['et0'])
                if o == 0:
                    tt('dve', Z1t[:, j, :], et[0], xg[g2], ALU.mult, ['et0', 'xg%d' % g2], ['Z1t'])
                else:
                    tt('dve', et[1], et[0], xg[g2], ALU.mult, ['et0', 'xg%d' % g2], ['et1'])
                    act(et[2], et[1], AF.Square, ['et1'], ['et2', 'ssb'], accum=ssb[:, j:j + 1])
                    cp('dve', Ut[:, j, :], et[1], ['et1'], ['Ut'])
        chk('conv')
        act(rsb, ssb, AF.Sqrt, ['ssb', 'cst'], ['rsb'], scale=1.0 / 512.0, bias=eps_c)
        fw.op('dve', lambda e: e.reciprocal(out=rstd_b, in_=rsb), ['rsb'], ['rstd_b'])
        for c in range(4):
            for h in range(2):
                b = bank()
                psb = psum[:, b, :].bitcast(BF16)
                for i8 in range(8):
                    i = 8 * h + i8
                    tr(psb[:, 128 * i8:128 * i8 + 128], Ut[:, i, 128 * c:128 * c + 128], ident_b,
                       ['Ut', 'ident_b'], ['ps%d' % b])
                ts('dve' if h == 0 else 'dve', ybst[0][:, 1024 * h:1024 * h + 1024], psb,
                   cols[:, R_ONB + c:R_ONB + c + 1], None, ALU.mult, None, ['ps%d' % b, 'cols'], ['ybst'])
            dma('sp', yb_s[c], ybst[0], ['ybst'], [('yb_s', c)])
        if debug:
            dma('sp', dbg_s[2][:, 0:512], rnorm, ['rnorm'], ['dbg2'])
        fw.barrier()


        chk('B')
        Cq = Alloc(arena, P.end, NW)
        X1 = Cq.f32([128, NT, D])
        h2T = Cq.bf16([128, 8, T])
        Lg = Cq.f32([128, NT, 36])
        comb = Cq.f32([128, NT, 32])
        fing = Cq.f32([128, D])
        wr = Cq.f32([128, 8, 36])
        br = Cq.f32([1, 36])
        ss2 = Cq.f32([128, 16])
        rs2 = Cq.f32([128, 16])
        rstd2 = Cq.f32([128, 16])
        wmark = Cq.off
        wg = [Cq.bf16([128, 8, DE]) for _ in range(2)]
        wu = [Cq.bf16([128, 8, DE]) for _ in range(2)]
        wd = [Cq.bf16([128, 4, D]) for _ in range(2)]
        tmark = Cq.off
        acth = [Cq.bf16([128, 4, 512]) for _ in range(2)]
        sg = [Cq.f32([128, 512]) for _ in range(2)]
        junkf = Cq.bf16([128, D])
        Mq = Alloc(arena, wmark, NW)
        yab = Mq.bf16([128, 8, T])
        wo = Mq.bf16([128, 8, D])
        xt2 = [Mq.f32([128, D]) for _ in range(3)]
        yts = [Mq.f32([128, D]) for _ in range(2)]
        h2f = [Mq.f32([128, 8, 128]) for _ in range(3)]
        junk2 = Mq.bf16([128, D])

        for c in range(4):
            dma('sp', yab[:, c, :], ya_s[c], [('ya_s', c)], [('yab', c)])
            dma('sp', yab[:, 4 + c, :], yb_s[c], [('yb_s', c)], [('yab', 4 + c)])
        dma('pool', wo, w_out_d.rearrange("(k p) n -> p k n", p=128), [], ['wo'])
        for k in range(8):
            dma('sp', wr[:, k, :], w_r_d[128 * k:128 * k + 128, :], [], ['wr'])
        dma('sp', br, b_r_d[:, :], [], ['br'])
        dma('sp', fing, final_g_d.partition_broadcast(128), [], ['fing'])
        chk('cdma')
        tt('dve', ssa[:, 0, :], ssa[:, 0, :], ssa[:, 1, :], ALU.add, ['ssa'], ['ssa'])
        tt('dve', ssa[:, 2, :], ssa[:, 2, :], ssa[:, 3, :], ALU.add, ['ssa'], ['ssa'])
        tt('dve', ssa[:, 0, :], ssa[:, 0, :], ssa[:, 2, :], ALU.add, ['ssa'], ['ssa'])
        act(ssa[:, 1, :], ssa[:, 0, :], AF.Sqrt, ['ssa', 'cst'], ['ssa'], scale=1.0 / 512.0, bias=eps_c)
        fw.op('dve', lambda e: e.reciprocal(out=rstd_a, in_=ssa[:, 1, :]), ['ssa'], ['rstd_a'])
        mset('dve', ss2, 0.0, ['ss2'])
        chk('crstd')
        st['nb'] = 4
        st['psrr'] = 0
        st['pair'] = 0

        def pair2():
            p = st.get('pair2', 0)
            st['pair2'] = 1 - p
            return 4 + 2 * p

        yabk = [('yab', c) for c in range(8)]
        g1v = g_rep[:, 0:2, :]
        g2v = g_rep[:, 2:4, :]
        def mg_stage1(i):
            s = i % 3
            xk = 'xt2%d' % s
            dma('sp', xt2[s], xpe_s[i], [('xpe', i)], [xk])
            pa = pair2()
            pbk = pair2()
            for half in range(2):
                for c in range(4):
                    mm(psum[:, pa + half, :], yab[:, c, 128 * i:128 * i + 128], wo[:, c, 512 * half:512 * half + 512],
                       c == 0, c == 3, yabk + ['wo'], ['ps%d' % (pa + half)])
                for c in range(4):
                    mm(psum[:, pbk + half, :], yab[:, 4 + c, 128 * i:128 * i + 128],
                       wo[:, 4 + c, 512 * half:512 * half + 512], c == 0, c == 3, yabk + ['wo'], ['ps%d' % (pbk + half)])
            yt_ = yts[i % 2]
            yk = 'yt%d' % (i % 2)
            ytv = yt_.rearrange("p (a b) -> p a b", a=2)
            act(ytv, psum[:, pa:pa + 2, :], AF.Copy, ['ps%d' % pa, 'ps%d' % (pa + 1), 'rstd_a'], [yk],
                scale=rstd_a[:, i:i + 1])
            stt('dve', ytv, psum[:, pbk:pbk + 2, :], rstd_b[:, i:i + 1], ytv, ALU.mult, ALU.add,
                ['ps%d' % pbk, 'ps%d' % (pbk + 1), 'rstd_b', yk], [yk])
            tt('dve', ytv, ytv, g1v, ALU.mult, [yk, 'g_rep'], [yk])
            X1i = X1[:, i, :]
            tt('dve', X1i, xt2[s], yt_, ALU.add, [xk, yk], [('X1', i)])
            act(junk2, X1i, AF.Square, [('X1', i)], ['junk2', 'ss2'], accum=ss2[:, i:i + 1])
            act(rs2[:, i:i + 1], ss2[:, i:i + 1], AF.Sqrt, ['ss2', 'cst'], ['rs2'], scale=1.0 / D, bias=eps_c)
            fw.op('dve', lambda e, i=i: e.reciprocal(out=rstd2[:, i:i + 1], in_=rs2[:, i:i + 1]), ['rs2'], ['rstd2'])
            ts('dve', xt2[s], X1i, rstd2[:, i:i + 1], None, ALU.mult, None, [('X1', i), 'rstd2', xk], [xk])

        def mg_stage2(i):
            s = i % 3
            xk = 'xt2%d' % s
            pb = 2 * (i % 2)
            pk = ['ps%d' % pb, 'ps%d' % (pb + 1)]
            psT = psum[:, pb:pb + 2, :].rearrange("p a (b c) -> p (a b) c", c=128)
            for k in range(8):
                tr(psT[:, k, :], xt2[s][:, 128 * k:128 * k + 128], ident_f, [xk, 'ident_f'], pk)
            hk = 'h2f%d' % s
            for k in range(8):
                if k < 4:
                    act(h2f[s][:, k, :], psT[:, k, :], AF.Identity, [pk[0], 'scl2', 'modc'], [hk],
                        scale=scl2[:, k:k + 1], bias=modc[:, 16 + k, 0:1])
                else:
                    ts('dve', h2f[s][:, k, :], psT[:, k, :], scl2[:, k:k + 1], modc[:, 16 + k, 0:1], ALU.mult, ALU.add,
                       [pk[1], 'scl2', 'modc'], [hk])
            cp('dve', h2T[:, :, 128 * i:128 * i + 128], h2f[s], [hk], [('h2T', i)])

        def mg_stage2b(i):
            s = i % 3
            hk = 'h2f%d' % s
            b = 2 * (i % 2)
            for k in range(8):
                mm(psum[:, b, 0:36], h2f[s][:, k, :], wr[:, k, :], k == 0, False, [hk, 'wr'], ['ps%d' % b])
            mm(psum[:, b, 0:36], ones_f[0:1, :], br[0:1, :], False, True, ['ones_f', 'br'], ['ps%d' % b])
            cp('dve', Lg[:, i, :], psum[:, b, 0:36], ['ps%d' % b], ['Lg'])

        mg_stage1(0)
        mg_stage1(1)
        for i in range(NT):
            mg_stage2(i)
            if i + 2 < NT:
                mg_stage1(i + 2)
            mg_stage2b(i)
        if debug:
            dma('sp', dbg_s[3][:, 0:576], Lg.rearrange("p a b -> p (a b)"), ['Lg'], ['dbg3'])
        fw.barrier()

        chk('merge')
        wgv = w_gate_d.rearrange("e (k p) n -> e p k n", p=128)
        wuv = w_up_d.rearrange("e (k p) n -> e p k n", p=128)
        wdv = w_down_d.rearrange("e (k p) n -> e p k n", p=128)

        def load_expert(e_):
            if e_ >= NE:
                return
            s = e_ % 2
            dma('pool', wg[s], wgv[e_], [], ['wg%d' % s])
            dma('pool', wu[s], wuv[e_], [], ['wu%d' % s])
            dma('pool', wd[s], wdv[e_], [], ['wd%d' % s])

        load_expert(0)
        load_expert(1)
        Rq = Alloc(arena, Cq.off, NW)
        gmax = Rq.f32([128, NT])
        goh = Rq.f32([128, NT, 4])
        gex = Rq.f32([128, NT, 4])
        gsum = Rq.f32([128, NT])
        gp = Rq.f32([128, NT])
        esel = Rq.f32([128, NT, 8])
        etmp = Rq.f32([128, NT, 8])
        oh1 = Rq.f32([128, NT, 8])
        oh2 = Rq.f32([128, NT, 8])
        msk = Rq.f32([128, NT, 8])
        m1 = Rq.f32([128, NT])
        m2 = Rq.f32([128, NT])
        dlt = Rq.f32([128, NT])
        w1 = Rq.f32([128, NT])
        w2 = Rq.f32([128, NT])
        c8 = Rq.f32([128, NT, 8])
        gl = Lg[:, :, 0:4]
        el = Lg[:, :, 4:36].rearrange("p a (g e) -> p a g e", g=4)

        def bc(ap, n):
            return ap.unsqueeze(2).to_broadcast([128, NT, n])

        def dv(fn, r, w):
            fw.op('dve', fn, r, w)
        dv(lambda e: e.tensor_reduce(out=gmax, in_=gl, axis=AX.X, op=ALU.max), ['Lg'], ['gmax'])
        tt('dve', goh, gl, bc(gmax, 4), ALU.is_equal, ['Lg', 'gmax'], ['goh'])
        tt('dve', gex, gl, bc(gmax, 4), ALU.subtract, ['Lg', 'gmax'], ['gex'])
        act(gex, gex, AF.Exp, ['gex'], ['gex'])
        dv(lambda e: e.tensor_reduce(out=gsum, in_=gex, axis=AX.X, op=ALU.add), ['gex'], ['gsum'])
        dv(lambda e: e.reciprocal(out=gp, in_=gsum), ['gsum'], ['gp'])
        tt('dve', esel, el[:, :, 0, :], bc(goh[:, :, 0], 8), ALU.mult, ['Lg', 'goh'], ['esel'])
        for g in range(1, 4):
            tt('dve', etmp, el[:, :, g, :], bc(goh[:, :, g], 8), ALU.mult, ['Lg', 'goh', 'esel'], ['etmp'])
            tt('dve', esel, esel, etmp, ALU.add, ['esel', 'etmp'], ['esel'])
        dv(lambda e: e.tensor_reduce(out=m1, in_=esel, axis=AX.X, op=ALU.max), ['esel'], ['m1'])
        tt('dve', oh1, esel, bc(m1, 8), ALU.is_equal, ['esel', 'm1'], ['oh1'])
        stt('dve', msk, oh1, -1e30, esel, ALU.mult, ALU.add, ['oh1', 'esel'], ['msk'])
        dv(lambda e: e.tensor_reduce(out=m2, in_=msk, axis=AX.X, op=ALU.max), ['msk'], ['m2'])
        tt('dve', oh2, msk, bc(m2, 8), ALU.is_equal, ['msk', 'm2'], ['oh2'])
        tt('dve', dlt, m2, m1, ALU.subtract, ['m1', 'm2'], ['dlt'])
        act(dlt, dlt, AF.Exp, ['dlt'], ['dlt'])
        ts('dve', w1, dlt, 1.0, None, ALU.add, None, ['dlt'], ['w1'])
        dv(lambda e: e.reciprocal(out=w1, in_=w1), ['w1'], ['w1'])
        tt('dve', w1, w1, gp, ALU.mult, ['w1', 'gp'], ['w1'])
        tt('dve', w2, w1, dlt, ALU.mult, ['w1', 'dlt'], ['w2'])
        tt('dve', c8, oh1, bc(w1, 8), ALU.mult, ['oh1', 'w1'], ['c8'])
        tt('dve', etmp, oh2, bc(w2, 8), ALU.mult, ['oh2', 'w2', 'esel'], ['etmp'])
        tt('dve', c8, c8, etmp, ALU.add, ['c8', 'etmp'], ['c8'])
        comb4 = comb.rearrange("p a (g e) -> p a g e", g=4)
        for g in range(4):
            tt('dve', comb4[:, :, g, :], c8, bc(goh[:, :, g], 8), ALU.mult, ['c8', 'goh'], ['comb'])
        if debug:
            dma('sp', dbg_s[4][:, 0:512], comb.rearrange("p a b -> p (a b)"), ['comb'], ['dbg4'])

        chk('route')
        outk = []

        def final_tile(i):
            X1i = X1[:, i, :]
            act(junkf, X1i, AF.Square, [('X1', i)], ['junkf', 'ss2'], accum=ss2[:, i:i + 1])
            act(rs2[:, i:i + 1], ss2[:, i:i + 1], AF.Sqrt, ['ss2', 'cst'], ['rs2'], scale=1.0 / D, bias=eps_c)
            fw.op('dve', lambda e, i=i: e.reciprocal(out=rstd2[:, i:i + 1], in_=rs2[:, i:i + 1]), ['rs2'], ['rstd2'])
            stt('dve', X1i, X1i, rstd2[:, i:i + 1], fing, ALU.mult, ALU.mult, [('X1', i), 'rstd2', 'fing'], [('X1', i)])
            dma('sp', out_d[128 * i:128 * i + 128, :], X1i, [('X1', i)], [('out', i)])
            outk.append(('out', i))

        mset('dve', ss2, 0.0, ['ss2'])
        h2k = [[('h2T', 4 * n + j) for j in range(4)] for n in range(4)]
        st['nb'] = 4
        st['psrr'] = 0
        ai = 0
        pending = []

        def flush():
            while pending:
                pending.pop(0)()

        for e_ in range(NE):
            s = e_ % 2
            for f in range(4):
                tt('dve', wd[s][:, f, :].rearrange("p (a b) -> p a b", a=2), wd[s][:, f, :].rearrange("p (a b) -> p a b", a=2),
                   g2v, ALU.mult, ['wd%d' % s, 'g_rep'], ['wd%d' % s])
            for n in range(4):
                a = ai % 2
                ai += 1
                ak = 'acth%d' % a
                for f in range(4):
                    bg = bank()
                    for k in range(8):
                        mm(psum[:, bg, :], wg[s][:, k, 128 * f:128 * f + 128], h2T[:, k, 512 * n:512 * n + 512],
                           k == 0, k == 7, ['wg%d' % s] + h2k[n], ['ps%d' % bg])
                    bu = bank()
                    for k in range(8):
                        mm(psum[:, bu, :], wu[s][:, k, 128 * f:128 * f + 128], h2T[:, k, 512 * n:512 * n + 512],
                           k == 0, k == 7, ['wu%d' % s] + h2k[n], ['ps%d' % bu])
                    r_ = f % 2
                    act(sg[r_], psum[:, bg, :], AF.Silu, ['ps%d' % bg], ['sg%d' % r_])
                    tt('dve', acth[a][:, f, :], sg[r_], psum[:, bu, :], ALU.mult, ['sg%d' % r_, 'ps%d' % bu], [ak])
                    if f == 0:
                        flush()

                def down(e_=e_, s=s, n=n, a=a, ak=ak):
                    for ti in range(4):
                        i = 4 * n + ti
                        pd = pair2()
                        for half in range(2):
                            for f in range(4):
                                mm(psum[:, pd + half, :], acth[a][:, f, 128 * ti:128 * ti + 128],
                                   wd[s][:, f, 512 * half:512 * half + 512], f == 0, f == 3,
                                   [ak, 'wd%d' % s], ['ps%d' % (pd + half)])
                        X1v = X1[:, i, :].rearrange("p (a b) -> p a b", a=2)
                        stt('dve', X1v, psum[:, pd:pd + 2, :], comb[:, i, e_:e_ + 1], X1v, ALU.mult, ALU.add,
                            ['ps%d' % pd, 'ps%d' % (pd + 1), 'comb', ('X1', i)], [('X1', i)])
                        if e_ == NE - 1:
                            final_tile(i)
                    if n == 3:
                        load_expert(e_ + 2)
                pending.append(down)
        flush()

        chk('moe')
        fw.op('sp', None, reads=outk)

        counts = fw.emit()
    return nc, counts


_PROG = {}


def _get_program(debug=False, stop=None):
    if (debug, stop) not in _PROG:
        _PROG[(debug, stop)] = build_program(debug, stop)
    return _PROG[(debug, stop)]


def _core_inputs(inp, b):
    f = lambda a: np.ascontiguousarray(np.asarray(a, dtype=np.float32))
    rows = np.concatenate([
        f(inp["c"])[b].reshape(8, 128),
        f(inp["c_ctx"]).reshape(8, 128),
        f(inp["b_ada"])[0].reshape(48, 128),
        f(inp["norm1_g"])[0].reshape(8, 128),
        f(inp["norm2_g"])[0].reshape(8, 128),
        f(inp["conv_a_w"])[0].reshape(16, 128),
        f(inp["conv_a_b"])[0].reshape(4, 128),
        f(inp["lru_ba"])[0].reshape(8, 128),
        f(inp["lru_bx"])[0].reshape(8, 128),
        f(inp["lru_lambda"])[0].reshape(8, 128),
        f(inp["conv_b_w"])[0].reshape(36, 128),
        f(inp["out_norm_a"])[0].reshape(4, 128),
        f(inp["out_norm_b"])[0].reshape(4, 128),
    ], axis=0)
    assert rows.shape == (NR, 128)
    return {
        "x": f(inp["x"])[b],
        "ctx": f(inp["ctx"])[b],
        "rows": f(rows),
        "w_ada": f(inp["w_ada"])[0],
        "b_ada": f(inp["b_ada"])[0].reshape(1, 6 * D),
        "w_in": f(inp["w_in"])[0],
        "lru_wa": f(inp["lru_wa"])[0].reshape(16, 64, 64),
        "lru_wx": f(inp["lru_wx"])[0].reshape(16, 64, 64),
        "filt_w1": f(inp["filt_w1"])[0],
        "filt_cols": f(np.stack([f(inp["filt_b1"])[0], f(inp["filt_freq1"])[0],
                                 f(inp["filt_b2"])[0], f(inp["filt_freq2"])[0]], axis=1)),
        "filt_w2": f(inp["filt_w2"])[0],
        "filt_w3": f(inp["filt_w3"])[0],
        "filt_b3": f(inp["filt_b3"])[0].reshape(1, 2048),
        "filt_bias": f(inp["filt_bias"])[0].reshape(1, 1024),
        "w_out": f(inp["w_out"])[0],
        "w_r": f(np.concatenate([f(inp["w_rg"])[0], f(inp["w_re"])[0]], axis=1)),
        "b_r": f(np.concatenate([f(inp["b_rg"])[0], f(inp["b_re"])[0]], axis=0)).reshape(1, 36),
        "w_gate": f(inp["w_gate"])[0],
        "w_up": f(inp["w_up"])[0],
        "w_down": f(inp["w_down"])[0],
        "final_g": f(inp["final_g"]).reshape(1, D),
    }


def kernel(**inputs):
    nc, _ = _get_program(False)
    nb = int(np.asarray(inputs["x"]).shape[0])
    shared = None
    in_maps = []
    for b in range(nb):
        m = _core_inputs(inputs, b)
        if shared is None:
            shared = m
        else:
            for k in m:
                if k not in ("x", "ctx", "rows"):
                    m[k] = shared[k]
        in_maps.append(m)
    res = run_bass_kernel_spmd(nc, in_maps, core_ids=list(range(nb)))
    return np.stack([np.asarray(r["out"], dtype=np.float32) for r in res.results], axis=0)
```
